# Optimizing a Trainium2 kernel written in Bass

```python
import jax, jax.numpy as jnp
from jax import lax
import numpy as np

D_MODEL = 2048
BATCH = 4
SEQ = 4096
DEPTH = 2

MIX_WIDTH = D_MODEL
FOURIER_WIDTH = MIX_WIDTH // 2
POOL_WIDTH = MIX_WIDTH - FOURIER_WIDTH
N_FOURIER_HEADS = 4
FOURIER_HEAD_DIM = FOURIER_WIDTH // N_FOURIER_HEADS
POOL_WINDOWS = (2, 4, 8, 16)
N_POOL_GROUPS = len(POOL_WINDOWS)
POOL_GROUP_DIM = POOL_WIDTH // N_POOL_GROUPS
N_EXPERTS = 16
CAPACITY_FACTOR = 2
D_EXPERT = D_MODEL
RMS_EPS = 1e-6

kernel_name = "fourier_pool_hybrid_ec_moe_encoder"


def rms_norm(x, g):
    xf = x.astype(jnp.float32)
    inv = lax.rsqrt(jnp.mean(xf * xf, axis=-1, keepdims=True) + RMS_EPS)
    return (xf * inv).astype(x.dtype) * g


def centred_pool_minus_identity(u, window):
    b, l, c = u.shape
    cs = jnp.concatenate([jnp.zeros((b, 1, c), u.dtype), jnp.cumsum(u, axis=1)], axis=1)
    t = jnp.arange(l)
    lo = jnp.clip(t - window // 2, 0, l)
    hi = jnp.clip(t + window - window // 2, 0, l)
    win_sum = jnp.take(cs, hi, axis=1) - jnp.take(cs, lo, axis=1)
    count = (hi - lo).astype(u.dtype)[None, :, None]
    return win_sum / count - u


def token_mixer(h, w_in, w_fourier, w_pool, pool_scale, w_out):
    b, l, _ = h.shape
    u = h @ w_in
    ua = u[..., :FOURIER_WIDTH].reshape(b, l, N_FOURIER_HEADS, FOURIER_HEAD_DIM)
    ub = u[..., FOURIER_WIDTH:].reshape(b, l, N_POOL_GROUPS, POOL_GROUP_DIM)
    fa = jnp.fft.fftn(ua.astype(jnp.float32), axes=(1, 3), norm="ortho").real.astype(h.dtype)
    ya = jnp.einsum('blgc,gcd->blgd', fa, w_fourier)
    pooled = jnp.stack(
        [centred_pool_minus_identity(ub[:, :, gi].astype(jnp.float32), w)
         for gi, w in enumerate(POOL_WINDOWS)], axis=2).astype(h.dtype)
    yb = jnp.einsum('blgc,gcd->blgd', pooled, w_pool) * pool_scale
    y = jnp.concatenate([ya.reshape(b, l, FOURIER_WIDTH), yb.reshape(b, l, POOL_WIDTH)], axis=-1)
    return y @ w_out


def expert_choice_ffn(h, w_router, w_gate, w_up, w_down):
    b, l, d = h.shape
    cap = CAPACITY_FACTOR * l // N_EXPERTS
    probs = jax.nn.softmax((h @ w_router).astype(jnp.float32), axis=-1)
    gates, idx = lax.top_k(jnp.swapaxes(probs, 1, 2), cap)
    flat = (idx + (jnp.arange(b) * l)[:, None, None]).reshape(-1)
    xs = jnp.take(h.reshape(b * l, d), flat, axis=0).reshape(b, N_EXPERTS, cap, d)
    hid = jax.nn.silu(jnp.einsum('becd,edf->becf', xs, w_gate)) * jnp.einsum('becd,edf->becf', xs, w_up)
    ys = jnp.einsum('becf,efd->becd', hid, w_down) * gates[..., None].astype(h.dtype)
    out = jnp.zeros((b * l, d), h.dtype).at[flat].add(ys.reshape(-1, d))
    return out.reshape(b, l, d)


def setup_inputs(seed: int = 0) -> dict:
    key = jax.random.key(seed)
    ks = jax.random.split(key, 14)
    f32 = jnp.float32
    def nrm(k, shape, fan_in):
        return jax.random.normal(k, shape, f32) * (fan_in ** -0.5)
    x = jax.random.normal(ks[0], (BATCH, SEQ, D_MODEL), f32)
    norm1_g = 1.0 + 0.02 * jax.random.normal(ks[1], (DEPTH, D_MODEL), f32)
    w_in = nrm(ks[2], (DEPTH, D_MODEL, MIX_WIDTH), D_MODEL)
    w_fourier = nrm(ks[3], (DEPTH, N_FOURIER_HEADS, FOURIER_HEAD_DIM, FOURIER_HEAD_DIM), FOURIER_HEAD_DIM)
    w_pool = nrm(ks[4], (DEPTH, N_POOL_GROUPS, POOL_GROUP_DIM, POOL_GROUP_DIM), POOL_GROUP_DIM)
    pool_scale = 1.0 + 0.02 * jax.random.normal(ks[5], (DEPTH, N_POOL_GROUPS, POOL_GROUP_DIM), f32)
    w_out = nrm(ks[6], (DEPTH, MIX_WIDTH, D_MODEL), MIX_WIDTH)
    norm2_g = 1.0 + 0.02 * jax.random.normal(ks[7], (DEPTH, D_MODEL), f32)
    w_router = nrm(ks[8], (DEPTH, D_MODEL, N_EXPERTS), D_MODEL)
    w_gate = nrm(ks[9], (DEPTH, N_EXPERTS, D_MODEL, D_EXPERT), D_MODEL)
    w_up = nrm(ks[10], (DEPTH, N_EXPERTS, D_MODEL, D_EXPERT), D_MODEL)
    w_down = nrm(ks[11], (DEPTH, N_EXPERTS, D_EXPERT, D_MODEL), D_EXPERT)
    final_g = 1.0 + 0.02 * jax.random.normal(ks[12], (D_MODEL,), f32)
    return {"x": x, "norm1_g": norm1_g, "w_in": w_in, "w_fourier": w_fourier,
            "w_pool": w_pool, "pool_scale": pool_scale, "w_out": w_out,
            "norm2_g": norm2_g, "w_router": w_router, "w_gate": w_gate,
            "w_up": w_up, "w_down": w_down, "final_g": final_g}


def reference(x, norm1_g, w_in, w_fourier, w_pool, pool_scale, w_out,
              norm2_g, w_router, w_gate, w_up, w_down, final_g):
    for layer in range(DEPTH):
        h = rms_norm(x, norm1_g[layer])
        x = x + token_mixer(h, w_in[layer], w_fourier[layer], w_pool[layer],
                            pool_scale[layer], w_out[layer])
        h = rms_norm(x, norm2_g[layer])
        x = x + expert_choice_ffn(h, w_router[layer], w_gate[layer], w_up[layer], w_down[layer])
    return rms_norm(x, final_g)
```

```python
import numpy as np
import ml_dtypes
from contextlib import ExitStack
import concourse.bass as bass
import concourse.mybir as mybir
from concourse.bass_utils import run_bass_kernel_spmd

F32 = mybir.dt.float32
BF16 = mybir.dt.bfloat16
U32 = mybir.dt.uint32
ALU = mybir.AluOpType
AF = mybir.ActivationFunctionType
AX = mybir.AxisListType

L = 4096
D = 2048
NL = 2
NE = 16
CAP = 512
NT = L // 128
EPS = 1e-6
NCORES = 4


class Tl:
    __slots__ = ("name", "w", "r", "rd")

    def __init__(self, name):
        self.name = name
        self.w = None
        self.r = {}
        self.rd = []


class Op:
    __slots__ = ("eng", "fn", "dma", "waits", "idx", "needs_inc", "sem", "val", "prev", "incval")


class Sched:
    ENGS = ("pe", "act", "dve", "pool", "sp")

    def __init__(self, nc, csem, pools):
        self.nc = nc
        self.csem = csem
        self.pools = pools
        self.ops = {e: [] for e in self.ENGS}
        self.waited = {e: {} for e in self.ENGS}
        self.dma_waited = {e: set() for e in self.ENGS}
        self.dma_count = {e: 0 for e in self.ENGS}
        self.last_compute = {e: None for e in self.ENGS}
        self.outstanding = []
        self.tiles = []

    def tile(self, name):
        t = Tl(name)
        self.tiles.append(t)
        return t

    def add(self, eng, fn, reads=(), writes=(), dma=False):
        op = Op()
        op.eng = eng
        op.fn = fn
        op.dma = dma
        op.idx = len(self.ops[eng])
        op.needs_inc = False
        op.sem = None
        op.val = 0
        op.prev = None
        op.incval = 0
        deps = []
        for t in reads:
            if t.w is not None:
                deps.append(t.w)
        for t in writes:
            if t.w is not None:
                deps.append(t.w)
            deps.extend(t.r.values())
            deps.extend(t.rd)
        waits = []
        for d in deps:
            if d is op:
                continue
            if d.dma:
                if d in self.dma_waited[eng]:
                    continue
                self.dma_waited[eng].add(d)
                waits.append(d)
            else:
                if eng == "pe" and d.eng == "pe" and not dma:
                    continue
                if self.waited[eng].get(d.eng, -1) >= d.idx:
                    continue
                self.waited[eng][d.eng] = d.idx
                d.needs_inc = True
                waits.append(d)
        if dma:
            pool = self.pools[eng]
            k = self.dma_count[eng]
            op.sem = pool[k % len(pool)]
            uses = k // len(pool)
            op.val = 16 * (uses + 1)
            op.prev = (op.sem, 16 * uses) if uses > 0 else None
            self.dma_count[eng] += 1
            self.outstanding.append(op)
        else:
            self.last_compute[eng] = op
        op.waits = waits
        for t in reads:
            if dma:
                t.rd.append(op)
            else:
                t.r[eng] = op
        for t in writes:
            t.w = op
            t.r = {}
            t.rd = []
        self.ops[eng].append(op)
        return op

    def barrier(self):
        best = {}
        for d in self.outstanding:
            key = id(d.sem)
            if key not in best or best[key].val < d.val:
                best[key] = d
        for eng in self.ENGS:
            waits = []
            for e2 in self.ENGS:
                j = self.last_compute[e2]
                if j is None:
                    continue
                if self.waited[eng].get(e2, -1) >= j.idx:
                    continue
                self.waited[eng][e2] = j.idx
                j.needs_inc = True
                waits.append(j)
            for d in best.values():
                waits.append(d)
            op = Op()
            op.eng = eng
            op.fn = None
            op.dma = False
            op.idx = len(self.ops[eng])
            op.needs_inc = False
            op.sem = None
            op.val = 0
            op.prev = None
            op.incval = 0
            op.waits = waits
            self.ops[eng].append(op)
            self.dma_waited[eng] = set()
        self.outstanding = []
        for t in self.tiles:
            t.w = None
            t.r = {}
            t.rd = []

    def emit(self, block):
        for eng in self.ENGS:
            c = 0
            for op in self.ops[eng]:
                if op.needs_inc:
                    c += 1
                    op.incval = c

        def run(eng, e):
            for op in self.ops[eng]:
                w = {}
                if op.dma and op.prev is not None:
                    w[id(op.prev[0])] = (op.prev[0], op.prev[1])
                for d in op.waits:
                    if d.dma:
                        s, v = d.sem, d.val
                    else:
                        s, v = self.csem[d.eng], d.incval
                    if id(s) not in w or w[id(s)][1] < v:
                        w[id(s)] = (s, v)
                for s, v in w.values():
                    e.wait_ge(s, v)
                if op.fn is not None:
                    ins = op.fn(e)
                    if op.dma:
                        ins.then_inc(op.sem, 16)
                    elif op.needs_inc:
                        ins.then_inc(self.csem[eng], 1)

        block.tensor(lambda e: run("pe", e))
        block.scalar(lambda e: run("act", e))
        block.vector(lambda e: run("dve", e))
        block.gpsimd(lambda e: run("pool", e))
        block.sync(lambda e: run("sp", e))


class Ring:
    def __init__(self, items):
        self.items = items
        self.k = 0

    def next(self):
        it = self.items[self.k % len(self.items)]
        self.k += 1
        return it


class Builder:
    def __init__(self, nlayers=NL, stop_after=None, debug=False, unit=None):
        self.unit = unit
        self.nlayers = nlayers
        self.stop_after = stop_after
        self.debug = debug

    def sb(self, es, name, shape, dt):
        self.uid = getattr(self, "uid", 0) + 1
        name = "%s_%d" % (name, self.uid)
        h = es.enter_context(self.nc.sbuf_tensor(name, shape, dt))
        return h, self.S.tile(name)

    def ps(self, es, name, shape, dt):
        self.uid = getattr(self, "uid", 0) + 1
        name = "%s_%d" % (name, self.uid)
        h = es.enter_context(self.nc.psum_tensor(name, shape, dt))
        return h, self.S.tile(name)

    def ring(self, es, name, n, shape, dt, psum=False):
        items = []
        for i in range(n):
            items.append((self.ps if psum else self.sb)(es, "%s%d" % (name, i), shape, dt))
        return Ring(items)

    def dma(self, eng, out, in_, reads, writes, **kw):
        return self.S.add(eng, lambda e: e.dma_start(out=out, in_=in_, **kw), reads, writes, dma=True)

    def mm(self, out, lhsT, rhs, start, stop, reads, writes):
        return self.S.add("pe", lambda e: e.matmul(out, lhsT, rhs, start=start, stop=stop), reads, writes)

    def tr(self, out, in_, ident, reads, writes):
        return self.S.add("pe", lambda e: e.transpose(out, in_, ident), reads, writes)

    def act(self, out, in_, func, reads, writes, **kw):
        return self.S.add("act", lambda e: e.activation(out=out, in_=in_, func=func, **kw), reads, writes)

    def copy(self, eng, out, in_, reads, writes):
        if eng == "act":
            return self.S.add("act", lambda e: e.copy(out=out, in_=in_), reads, writes)
        return self.S.add(eng, lambda e: e.tensor_copy(out=out, in_=in_), reads, writes)

    def tt(self, eng, out, in0, in1, op, reads, writes):
        return self.S.add(eng, lambda e: e.tensor_tensor(out=out, in0=in0, in1=in1, op=op), reads, writes)

    def ts(self, eng, out, in0, s1, s2, op0, op1, reads, writes, accum_out=None):
        if op1 is None:
            return self.S.add(eng, lambda e: e.tensor_scalar(out=out, in0=in0, scalar1=s1, scalar2=None, op0=op0),
                              reads, writes)
        if accum_out is not None:
            return self.S.add(eng, lambda e: e.tensor_scalar(out=out, in0=in0, scalar1=s1, scalar2=s2, op0=op0,
                                                             op1=op1, accum_out=accum_out), reads, writes)
        return self.S.add(eng, lambda e: e.tensor_scalar(out=out, in0=in0, scalar1=s1, scalar2=s2, op0=op0, op1=op1),
                          reads, writes)

    def stt(self, out, in0, scalar, in1, op0, op1, reads, writes):
        return self.S.add("dve", lambda e: e.scalar_tensor_tensor(out=out, in0=in0, scalar=scalar, in1=in1,
                                                                  op0=op0, op1=op1), reads, writes)

    def memset(self, eng, ap, val, writes):
        return self.S.add(eng, lambda e: e.memset(ap, val), (), writes)

    def rstd_of(self, xt, Txt, junk, Tjunk, small):
        ss, Tss = small.next()
        sd, Tsd = small.next()
        rs, Trs = small.next()
        self.act(junk[:, :], xt[:, :], AF.Square, [Txt], [Tjunk, Tss], accum_out=ss[:, 0:1])
        self.act(sd[:, 0:1], ss[:, 0:1], AF.Sqrt, [Tss, self.Teps], [Tsd], scale=1.0 / D, bias=self.eps[:, 0:1])
        self.S.add("dve", lambda e: e.reciprocal(out=rs[:, 0:1], in_=sd[:, 0:1]), [Tsd], [Trs])
        return rs, Trs

    def build(self):
        nc = bass.Bass("TRN2", target_bir_lowering=False)
        self.nc = nc
        def dt_in(name, shape, dt=F32):
            if self.unit and not name.startswith("c_"):
                return None
            return nc.dram_tensor(name, shape, dt, kind="ExternalInput").ap()
        self.x = dt_in("x", [L, D])
        self.norm1_g = dt_in("norm1_g", [NL, D])
        self.w_in = dt_in("w_in", [NL, D, D])
        self.w_fourier = dt_in("w_fourier", [NL, 4, 256, 256])
        self.w_pool = dt_in("w_pool", [NL, 4, 256, 256])
        self.pool_scale = dt_in("pool_scale", [NL, 4, 256])
        self.w_out = dt_in("w_out", [NL, D, D])
        self.norm2_g = dt_in("norm2_g", [NL, D])
        self.w_router = dt_in("w_router", [NL, D, NE])
        self.w_gate = dt_in("w_gate", [NL, NE, D, D])
        self.w_up = dt_in("w_up", [NL, NE, D, D])
        self.w_down = dt_in("w_down", [NL, NE, D, D])
        self.final_g = dt_in("final_g", [1, D])
        self.c_identb = dt_in("c_identb", [128, 128], BF16)
        self.c_identf = dt_in("c_identf", [128, 128])
        self.c_cs = dt_in("c_cs", [256, 512], BF16)
        self.c_dftc = dt_in("c_dftc", [8, 128, 32 * 256], BF16)
        self.c_dfts = dt_in("c_dfts", [8, 128, 32 * 256], BF16)
        self.c_nyq = dt_in("c_nyq", [128, 32 * 2], BF16)
        self.c_invedge = dt_in("c_invedge", [1, 4 * 16])
        self.c_blk = dt_in("c_blk", [128, 128])
        self.c_ltri = dt_in("c_ltri", [128, 128])
        self.c_io16 = dt_in("c_io16", [128, NE * 128])
        self.c_tokf = dt_in("c_tokf", [128, NT * NE])
        self.out = nc.dram_tensor("out", [L, D] if not self.unit else [128, 8], F32, kind="ExternalOutput").ap()
        if self.unit:
            self.u_p = nc.dram_tensor("u_p", [128, NT * NE], F32, kind="ExternalInput").ap()
        self.xres = nc.dram_tensor("xres", [L, D], F32).ap()
        self.Gd = nc.dram_tensor("Gd", [4, L, 512], BF16).ap()
        self.uBd = nc.dram_tensor("uBd", [1024, L], F32).ap()
        self.yT = nc.dram_tensor("yT", [D, L], BF16).ap()
        self.h2d = nc.dram_tensor("h2d", [L, D], BF16).ap()
        if self.debug:
            self.dbg_yT = nc.dram_tensor("dbg_yT", [D, L], BF16, kind="ExternalOutput").ap()
            self.dbg_x = nc.dram_tensor("dbg_x", [L, D], F32, kind="ExternalOutput").ap()
            self.dbg_p = nc.dram_tensor("dbg_p", [128, NT * NE], F32, kind="ExternalOutput").ap()
            self.dbg_idx = nc.dram_tensor("dbg_idx", [128, NE * 4], U32, kind="ExternalOutput").ap()
            self.dbg_gate = nc.dram_tensor("dbg_gate", [128, NE * 4], F32, kind="ExternalOutput").ap()

        with ExitStack() as top:
            csem = {}
            for e in Sched.ENGS[:4]:
                csem[e] = top.enter_context(nc.semaphore("c_" + e))
            pools = {}
            for e, n in (("sp", 40), ("pool", 24), ("act", 12), ("dve", 12)):
                pools[e] = [top.enter_context(nc.semaphore("d_%s%d" % (e, i))) for i in range(n)]
            self.S = Sched(nc, csem, pools)
            block = top.enter_context(nc.Block())
            with ExitStack() as es:
                self.identb, self.Tidentb = self.sb(es, "identb", [128, 128], BF16)
                self.identf, self.Tidentf = self.sb(es, "identf", [128, 128], F32)
                self.eps, self.Teps = self.sb(es, "eps", [128, 1], F32)
                self.dma("sp", self.identb[:, :], self.c_identb[:, :], [], [self.Tidentb])
                self.dma("sp", self.identf[:, :], self.c_identf[:, :], [], [self.Tidentf])
                self.memset("dve", self.eps[:, :], EPS, [self.Teps])
                if self.unit == "E":
                    self.unit_E(es)
                else:
                    self.body(es)
                self.S.barrier()
            self.S.emit(block)
        return nc

    def unit_E(self, es0):
        with ExitStack() as es:
            self.P_all, self.TP = self.sb(es, "P_all", [128, NT, NE], F32)
            self.slot_all, self.Tslot = self.sb(es, "slot_all", [128, NT, NE], F32)
            self.idx_all, self.Tidx = self.sb(es, "idx_all", [128, NE, 4], U32)
            self.gate_all, self.Tgate = self.sb(es, "gate_all", [128, NE, 4], F32)
            self.dma("sp", self.P_all[:, :, :], self.u_p.rearrange("p (i e) -> p i e", e=NE), [], [self.TP])
            self.phase_E(0)
            self.S.barrier()
            self.dma("sp", self.dbg_idx[:, :], self.idx_all[:, :, :], [self.Tidx], [])
            self.dma("sp", self.dbg_gate[:, :], self.gate_all[:, :, :], [self.Tgate], [])

    def body(self, es0):
        S = self.S
        for l in range(self.nlayers):
            xsrc = self.x if l == 0 else self.xres
            self.phase_A(l, xsrc)
            S.barrier()
            if self.stop_after == ("A", l):
                return
            self.phase_BC(l)
            S.barrier()
            if self.stop_after == ("C", l):
                self.dma("sp", self.dbg_yT[:, :], self.yT[:, :], [], [])
                return
            with ExitStack() as es:
                self.P_all, self.TP = self.sb(es, "P_all", [128, NT, NE], F32)
                self.slot_all, self.Tslot = self.sb(es, "slot_all", [128, NT, NE], F32)
                self.idx_all, self.Tidx = self.sb(es, "idx_all", [128, NE, 4], U32)
                self.gate_all, self.Tgate = self.sb(es, "gate_all", [128, NE, 4], F32)
                self.phase_D(l, xsrc)
                S.barrier()
                if self.stop_after == ("D", l):
                    self.dma("sp", self.dbg_x[:, :], self.xres[:, :], [], [])
                    self.dma("sp", self.dbg_p[:, :], self.P_all[:, :, :], [self.TP], [])
                    return
                esw = es.enter_context(ExitStack())
                self.wR = self.ring(esw, "wF", 7, [128, 16, 512], BF16)
                self.xsF = self.sb(esw, "xsF", [128, 4, D], BF16)
                self.pref = []
                units0 = [(self.w_gate[l, 0], 0), (self.w_up[l, 0], 0), (self.w_gate[l, 0], 1), (self.w_up[l, 0], 1),
                          (self.w_gate[l, 0], 2), (self.w_up[l, 0], 2)]
                for (src2d, blk) in units0:
                    wt, Twt = self.wR.next()
                    self.dma("pool", wt[:, :, :],
                             src2d.rearrange("(c p) f -> p c f", p=128)[:, :, blk * 512:(blk + 1) * 512], [], [Twt])
                    self.pref.append((wt, Twt))
                self.phase_E(l)
                S.barrier()
                if self.stop_after == ("E", l):
                    self.dma("sp", self.dbg_x[:, :], self.xres[:, :], [], [])
                    self.dma("sp", self.dbg_p[:, :], self.P_all[:, :, :], [self.TP], [])
                    self.dma("sp", self.dbg_idx[:, :], self.idx_all[:, :, :], [self.Tidx], [])
                    self.dma("sp", self.dbg_gate[:, :], self.gate_all[:, :, :], [self.Tgate], [])
                    return
                self.phase_F(l)
                S.barrier()
            if self.stop_after == ("F", l):
                self.dma("sp", self.dbg_x[:, :], self.xres[:, :], [], [])
                return
        self.phase_G()

    def phase_A(self, l, xsrc):
        S = self.S
        with ExitStack() as es:
            win, _ = self.sb(es, "win", [128, 16, D], BF16)
            Twin = [S.tile("winq%d" % q) for q in range(4)]
            gb, Tgb = self.sb(es, "gb", [128, D], F32)
            cs, Tcs = self.sb(es, "cs", [128, 2, 512], BF16)
            junk, Tjunk = self.sb(es, "junkA", [128, D], BF16)
            xtR = self.ring(es, "xtA", 3, [128, D], F32)
            hbR = self.ring(es, "hbA", 8, [128, D], BF16)
            hTR = self.ring(es, "hTA", 2, [128, 16, 512], BF16)
            uTfR = self.ring(es, "uTfA", 2, [128, 8, 512], BF16)
            upR = self.ring(es, "upA", 4, [128, 512], F32)
            gtR = self.ring(es, "gtA", 4, [128, 512], BF16)
            small = self.ring(es, "smA", 24, [128, 1], F32)
            ptR = self.ring(es, "ptA", 3, [128, 8, 128], BF16, psum=True)
            puR = self.ring(es, "puA", 3, [128, 512], F32, psum=True)
            pgR = self.ring(es, "pgA", 2, [128, 512], F32, psum=True)

            wsrc = self.w_in[l].rearrange("(c p) f -> p c f", p=128)
            for q in range(4):
                self.dma("pool", win[:, :, q * 512:(q + 1) * 512], wsrc[:, :, q * 512:(q + 1) * 512], [], [Twin[q]])
            self.dma("sp", gb[:, :], self.norm1_g[l].partition_broadcast(128), [], [Tgb])
            self.dma("sp", cs[:, :, :], self.c_cs.rearrange("(c p) f -> p c f", p=128), [], [Tcs])

            def norm(tb):
                hbs = []
                for i in range(4):
                    t0 = tb * 512 + i * 128
                    xt, Txt = xtR.next()
                    self.dma("sp", xt[:, :], xsrc[t0:t0 + 128, :], [], [Txt])
                    rs, Trs = self.rstd_of(xt, Txt, junk, Tjunk, small)
                    hb, Thb = hbR.next()
                    self.stt(hb[:, :], xt[:, :], rs[:, 0:1], gb[:, :], ALU.mult, ALU.mult, [Txt, Trs, Tgb], [Thb])
                    hbs.append((hb, Thb))
                return hbs

            def transp(hbs):
                hT, ThT = hTR.next()
                for i in range(4):
                    hb, Thb = hbs[i]
                    for half in range(2):
                        pt, Tpt = ptR.next()
                        for c in range(8):
                            cc = half * 8 + c
                            self.tr(pt[:, c, :], hb[:, cc * 128:(cc + 1) * 128], self.identb[:, :],
                                    [Thb, self.Tidentb], [Tpt])
                        self.copy("act" if half == 0 else "dve", hT[:, half * 8:(half + 1) * 8, i * 128:(i + 1) * 128],
                                  pt[:, :, :], [Tpt], [ThT])
                return hT, ThT

            self.evA = 0

            def mms(tb, hT, ThT):
                uTf, TuTf = uTfR.next()
                for cchunk in range(16):
                    pu, Tpu = puR.next()
                    for dc in range(16):
                        self.mm(pu[:, :], win[:, dc, cchunk * 128:(cchunk + 1) * 128], hT[:, dc, :],
                                dc == 0, dc == 15, [Twin[cchunk // 4], ThT], [Tpu])
                    if cchunk < 8:
                        self.copy("act" if cchunk % 2 == 0 else "dve", uTf[:, cchunk, :], pu[:, :], [Tpu], [TuTf])
                    else:
                        up, Tup = upR.next()
                        self.copy("act" if cchunk % 2 == 0 else "dve", up[:, :], pu[:, :], [Tpu], [Tup])
                        r0 = (cchunk - 8) * 128
                        self.dma("sp", self.uBd[r0:r0 + 128, tb * 512:(tb + 1) * 512], up[:, :], [Tup], [])
                for i in range(4):
                    for g in range(4):
                        pg, Tpg = pgR.next()
                        for cc in range(2):
                            self.mm(pg[:, :], uTf[:, 2 * g + cc, i * 128:(i + 1) * 128], cs[:, cc, :],
                                    cc == 0, cc == 1, [TuTf, Tcs], [Tpg])
                        gt, Tgt = gtR.next()
                        self.copy("act" if self.evA % 2 == 0 else "dve", gt[:, :], pg[:, :], [Tpg], [Tgt])
                        self.evA += 1
                        t0 = tb * 512 + i * 128
                        self.dma("sp", self.Gd[g, t0:t0 + 128, :], gt[:, :], [Tgt], [])

            hb0 = norm(0)
            hb1 = norm(1)
            cur = transp(hb0)
            nxt_hb = hb1
            for tb in range(8):
                hb2 = norm(tb + 2) if tb + 2 < 8 else None
                nxt = transp(nxt_hb) if tb + 1 < 8 else None
                mms(tb, cur[0], cur[1])
                cur = nxt
                nxt_hb = hb2

    def phase_BC(self, l):
        W = L + 32
        with ExitStack() as es:
            GsR = self.ring(es, "GsB", 1, [128, 32, 512], BF16)
            CbR = self.ring(es, "CbB", 2, [128, 32, 256], BF16)
            SbR = self.ring(es, "SbB", 2, [128, 32, 256], BF16)
            nyq, Tnyq = self.sb(es, "nyqB", [128, 32, 2], BF16)
            aR = self.ring(es, "aB", 2, [128, 256], F32)
            fpR = self.ring(es, "fpB", 2, [128, 2, 256], BF16)
            fmR = self.ring(es, "fmB", 2, [128, 2, 256], BF16)
            fan, Tfan = self.sb(es, "fanB", [128, 2, 2], BF16)
            ystR = self.ring(es, "ystB", 1, [128, 2, L], BF16)
            wfR = self.ring(es, "wfB", 2, [128, 2, 256], BF16)
            upR = self.ring(es, "upC", 1, [128, W], F32)
            ra, Tra = self.sb(es, "raC", [128, W], F32)
            rb, Trb = self.sb(es, "rbC", [128, W], F32)
            plR = self.ring(es, "plC", 1, [128, 2, L], BF16)
            ybR = self.ring(es, "ybC", 1, [128, 2, L], BF16)
            wpR = self.ring(es, "wpC", 2, [128, 2, 256], BF16)
            psc, Tpsc = self.sb(es, "pscC", [128, 4, 2], F32)
            ied, Tied = self.sb(es, "iedC", [128, 4, 16], F32)
            e1, Te1 = self.sb(es, "e1C", [128, 16], F32)
            paR = self.ring(es, "paB", 2, [128, 256], F32, psum=True)
            pbR = self.ring(es, "pbB", 2, [128, 256], F32, psum=True)
            pyR = self.ring(es, "pyB", 2, [128, 256], F32, psum=True)
            ppR = self.ring(es, "ppC", 2, [128, 512], F32, psum=True)

            for (u_, Tu_) in upR.items:
                self.memset("pool", u_[:, :], 0.0, [Tu_])
            self.memset("dve", ra[:, :], 0.0, [Tra])
            self.memset("dve", rb[:, :], 0.0, [Trb])
            self.dma("sp", nyq[:, :, :], self.c_nyq.rearrange("p (i k) -> p i k", k=2), [], [Tnyq])
            self.dma("sp", psc[:, :, :], self.pool_scale[l].rearrange("g (q p) -> p g q", p=128), [], [Tpsc],
                     allow_slow_non_contiguous=True)
            self.dma("sp", ied[:, :, :], self.c_invedge[0].partition_broadcast(128).rearrange("p (g k) -> p g k", k=16),
                     [], [Tied])

            def pool_ops(g, pl, Tpl):
                w = (2, 4, 8, 16)[g]
                ops = []
                lo, hi = 8, W - 8
                for cc in range(2):
                    up, Tup = upR.next()
                    r0 = (g * 2 + cc) * 128
                    ops.append(lambda up=up, Tup=Tup, r0=r0: self.dma("sp", up[:, 16:16 + L], self.uBd[r0:r0 + 128, :],
                                                                      [], [Tup]))
                    ops.append(lambda up=up, Tup=Tup: self.tt("dve", ra[:, lo:hi], up[:, lo - 1:hi - 1], up[:, lo:hi],
                                                              ALU.add, [Tup], [Tra]))
                    cur, Tcur, oth, Toth = ra, Tra, rb, Trb
                    sh = 1
                    for lev in range(g):
                        ops.append(lambda cur=cur, Tcur=Tcur, oth=oth, Toth=Toth, sh=sh: self.tt(
                            "dve", oth[:, lo:hi], cur[:, lo - sh:hi - sh], cur[:, lo + sh:hi + sh], ALU.add,
                            [Tcur], [Toth]))
                        cur, Tcur, oth, Toth = oth, Toth, cur, Tcur
                        sh *= 2
                    ops.append(lambda cur=cur, Tcur=Tcur, up=up, Tup=Tup, cc=cc: self.stt(
                        pl[:, cc, :], cur[:, 16:16 + L], 1.0 / w, up[:, 16:16 + L], ALU.mult, ALU.subtract,
                        [Tcur, Tup], [Tpl]))

                    def edges(cur=cur, Tcur=Tcur, up=up, Tup=Tup, cc=cc):
                        self.tt("dve", e1[:, 0:8], cur[:, 16:24], ied[:, g, 0:8], ALU.mult, [Tcur, Tied], [Te1])
                        self.tt("dve", e1[:, 8:16], cur[:, 16 + L - 8:16 + L], ied[:, g, 8:16], ALU.mult,
                                [Tcur, Tied], [Te1])
                        self.tt("dve", pl[:, cc, 0:8], e1[:, 0:8], up[:, 16:24], ALU.subtract, [Te1, Tup], [Tpl])
                        self.tt("dve", pl[:, cc, L - 8:L], e1[:, 8:16], up[:, 16 + L - 8:16 + L], ALU.subtract,
                                [Te1, Tup], [Tpl])
                    ops.append(edges)
                return ops

            def ystage(kb, fp, Tfp, fm, Tfm, wf, Twf, yst, Tyst):
                j0 = 1 if kb == 0 else 0
                mstart = L - kb * 256 - j0
                n = 256 - j0
                for dq in range(2):
                    py, Tpy = pyR.next()
                    for cq in range(2):
                        self.mm(py[:, :], wf[:, cq, dq * 128:(dq + 1) * 128], fp[:, cq, :], cq == 0, cq == 1,
                                [Twf, Tfp], [Tpy])
                    self.copy("act", yst[:, dq, kb * 256:(kb + 1) * 256], py[:, :], [Tpy], [Tyst])
                    py, Tpy = pyR.next()
                    for cq in range(2):
                        self.mm(py[:, :], wf[:, cq, dq * 128:(dq + 1) * 128], fm[:, cq, :], cq == 0, cq == 1,
                                [Twf, Tfm], [Tpy])
                    self.copy("act", yst[:, dq, mstart:mstart - n:-1], py[:, j0:256], [Tpy], [Tyst])

            pend = None
            for g in range(4):
                Gs, TGs = GsR.next()
                self.dma("sp", Gs[:, :, :], self.Gd[g].rearrange("(i p) f -> p i f", p=128), [], [TGs])
                wf, Twf = wfR.next()
                self.dma("pool", wf[:, :, :], self.w_fourier[l, g].rearrange("(c p) d -> p c d", p=128), [], [Twf])
                wp, Twp = wpR.next()
                self.dma("pool", wp[:, :, :], self.w_pool[l, g].rearrange("(c p) d -> p c d", p=128), [], [Twp])
                yst, Tyst = ystR.next()
                pl, Tpl = plR.next()
                cops = pool_ops(g, pl, Tpl)
                per_kb = (len(cops) + 7) // 8
                for kb in range(8):
                    Cb, TCb = CbR.next()
                    Sb, TSb = SbR.next()
                    self.dma("sp", Cb[:, :, :], self.c_dftc[kb].rearrange("p (i k) -> p i k", k=256), [], [TCb])
                    self.dma("sp", Sb[:, :, :], self.c_dfts[kb].rearrange("p (i k) -> p i k", k=256), [], [TSb])
                    fp, Tfp = fpR.next()
                    fm, Tfm = fmR.next()
                    for cq in range(2):
                        pa, Tpa = paR.next()
                        pb, Tpb = pbR.next()
                        for i in range(32):
                            self.mm(pa[:, :], Gs[:, i, cq * 128:(cq + 1) * 128], Cb[:, i, :], i == 0, i == 31,
                                    [TGs, TCb], [Tpa])
                        for i in range(32):
                            self.mm(pb[:, :], Gs[:, i, 256 + cq * 128:256 + (cq + 1) * 128], Sb[:, i, :], i == 0,
                                    i == 31, [TGs, TSb], [Tpb])
                        a_, Ta_ = aR.next()
                        self.copy("act", a_[:, :], pa[:, :], [Tpa], [Ta_])
                        self.tt("dve", fp[:, cq, :], a_[:, :], pb[:, :], ALU.add, [Ta_, Tpb], [Tfp])
                        self.tt("dve", fm[:, cq, :], a_[:, :], pb[:, :], ALU.subtract, [Ta_, Tpb], [Tfm])
                    if pend is not None:
                        ystage(*pend)
                    pend = (kb, fp, Tfp, fm, Tfm, wf, Twf, yst, Tyst)
                    for _ in range(per_kb):
                        if cops:
                            cops.pop(0)()
                ystage(*pend)
                pend = None
                while cops:
                    cops.pop(0)()
                for cq in range(2):
                    pa, Tpa = paR.next()
                    for i in range(32):
                        self.mm(pa[:, 0:2], Gs[:, i, cq * 128:(cq + 1) * 128], nyq[:, i, :], i == 0, i == 31,
                                [TGs, Tnyq], [Tpa])
                    self.copy("act", fan[:, cq, :], pa[:, 0:2], [Tpa], [Tfan])
                for dq in range(2):
                    py, Tpy = pyR.next()
                    for cq in range(2):
                        self.mm(py[:, 0:2], wf[:, cq, dq * 128:(dq + 1) * 128], fan[:, cq, :], cq == 0, cq == 1,
                                [Twf, Tfan], [Tpy])
                    self.copy("act", yst[:, dq, L // 2:L // 2 + 1], py[:, 0:1], [Tpy], [Tyst])
                self.dma("sp", self.yT[g * 256:(g + 1) * 256, :].rearrange("(q p) t -> p q t", p=128), yst[:, :, :],
                         [Tyst], [])
                yb, Tyb = ybR.next()
                for tb in range(8):
                    for dq in range(2):
                        pp, Tpp = ppR.next()
                        for cc in range(2):
                            self.mm(pp[:, :], wp[:, cc, dq * 128:(dq + 1) * 128], pl[:, cc, tb * 512:(tb + 1) * 512],
                                    cc == 0, cc == 1, [Twp, Tpl], [Tpp])
                        self.ts("dve", yb[:, dq, tb * 512:(tb + 1) * 512], pp[:, :], psc[:, g, dq:dq + 1], None,
                                ALU.mult, None, [Tpp, Tpsc], [Tyb])
                r0 = 1024 + g * 256
                self.dma("sp", self.yT[r0:r0 + 256, :].rearrange("(q p) t -> p q t", p=128), yb[:, :, :], [Tyb], [])

    def phase_D(self, l, xsrc):
        S = self.S
        with ExitStack() as es:
            wout, _ = self.sb(es, "wout", [128, 16, D], BF16)
            Twout = [S.tile("woutq%d" % q) for q in range(4)]
            gb, Tgb = self.sb(es, "gbD", [128, D], F32)
            wr, Twr = self.sb(es, "wrD", [128, 16, NE], F32)
            junk, Tjunk = self.sb(es, "junkD", [128, D], BF16)
            yTR = self.ring(es, "yTD", 2, [128, 16, 512], BF16)
            xtR = self.ring(es, "xtD", 2, [128, D], F32)
            xnR = self.ring(es, "xnD", 2, [128, D], F32)
            hfR = self.ring(es, "hfD", 2, [128, D], F32)
            hbR = self.ring(es, "hbD", 2, [128, D], BF16)
            hTR = self.ring(es, "hTD", 2, [128, 16, 128], F32)
            small = self.ring(es, "smD", 32, [128, 1], F32)
            exR = self.ring(es, "exD", 2, [128, NE], F32)
            poR = self.ring(es, "poD", 3, [128, 512], F32, psum=True)
            ptR = self.ring(es, "ptD", 4, [128, 4, 128], F32, psum=True)
            plR = self.ring(es, "plD", 1, [128, NE], F32, psum=True)

            wsrc = self.w_out[l].rearrange("(c p) f -> p c f", p=128)
            for q in range(4):
                self.dma("pool", wout[:, :, q * 512:(q + 1) * 512], wsrc[:, :, q * 512:(q + 1) * 512], [], [Twout[q]])
            self.dma("sp", gb[:, :], self.norm2_g[l].partition_broadcast(128), [], [Tgb])
            self.dma("sp", wr[:, :, :], self.w_router[l].rearrange("(c p) e -> p c e", p=128), [], [Twr])
            yview = self.yT.rearrange("(c p) t -> p c t", p=128)
            state = {"yTb": None}

            def stage1(ti):
                tb, i = ti // 4, ti % 4
                if i == 0:
                    yTb, TyTb = yTR.next()
                    self.dma("sp", yTb[:, :, :], yview[:, :, tb * 512:(tb + 1) * 512], [], [TyTb])
                    state["yTb"] = (yTb, TyTb)
                yTb, TyTb = state["yTb"]
                t0 = ti * 128
                xt, Txt = xtR.next()
                self.dma("sp", xt[:, :], xsrc[t0:t0 + 128, :], [], [Txt])
                xn, Txn = xnR.next()
                for db in range(4):
                    po, Tpo = poR.next()
                    for mc in range(16):
                        self.mm(po[:, :], yTb[:, mc, i * 128:(i + 1) * 128], wout[:, mc, db * 512:(db + 1) * 512],
                                mc == 0, mc == 15, [TyTb, Twout[db]], [Tpo])
                    self.tt("dve", xn[:, db * 512:(db + 1) * 512], po[:, :], xt[:, db * 512:(db + 1) * 512],
                            ALU.add, [Tpo, Txt], [Txn])
                self.dma("sp", self.xres[t0:t0 + 128, :], xn[:, :], [Txn], [])
                rs, Trs = self.rstd_of(xn, Txn, junk, Tjunk, small)
                hf, Thf = hfR.next()
                self.stt(hf[:, :], xn[:, :], rs[:, 0:1], gb[:, :], ALU.mult, ALU.mult, [Txn, Trs, Tgb], [Thf])
                hb, Thb = hbR.next()
                self.copy("act", hb[:, :], hf[:, :], [Thf], [Thb])
                self.dma("sp", self.h2d[t0:t0 + 128, :], hb[:, :], [Thb], [])
                return hf, Thf

            def stage2(hfp):
                hf, Thf = hfp
                hT, ThT = hTR.next()
                for q in range(4):
                    pt, Tpt = ptR.next()
                    for c in range(4):
                        cc = q * 4 + c
                        self.tr(pt[:, c, :], hf[:, cc * 128:(cc + 1) * 128], self.identf[:, :],
                                [Thf, self.Tidentf], [Tpt])
                    self.copy("act" if q % 2 == 0 else "dve", hT[:, q * 4:(q + 1) * 4, :], pt[:, :, :], [Tpt], [ThT])
                return hT, ThT

            def stage3(ti, hTp):
                hT, ThT = hTp
                pl, Tpl = plR.next()
                for dc in range(16):
                    self.mm(pl[:, :], hT[:, dc, :], wr[:, dc, :], dc == 0, dc == 15, [ThT, Twr], [Tpl])
                mx, Tmx = small.next()
                nmx, Tnmx = small.next()
                sm, Tsm = small.next()
                rsm, Trsm = small.next()
                ex, Tex = exR.next()
                self.S.add("dve", lambda e, mx=mx, pl=pl: e.reduce_max(out=mx[:, 0:1], in_=pl[:, :], axis=AX.X),
                           [Tpl], [Tmx])
                self.ts("dve", nmx[:, 0:1], mx[:, 0:1], -1.0, None, ALU.mult, None, [Tmx], [Tnmx])
                self.act(ex[:, :], pl[:, :], AF.Exp, [Tpl, Tnmx], [Tex, Tsm], bias=nmx[:, 0:1], scale=1.0,
                         accum_out=sm[:, 0:1])
                self.S.add("dve", lambda e, rsm=rsm, sm=sm: e.reciprocal(out=rsm[:, 0:1], in_=sm[:, 0:1]),
                           [Tsm], [Trsm])
                self.ts("dve", self.P_all[:, ti, :], ex[:, :], rsm[:, 0:1], None, ALU.mult, None, [Tex, Trsm],
                        [self.TP])

            hf_cur = stage1(0)
            hT_prev = None
            for ti in range(NT):
                hf_next = stage1(ti + 1) if ti + 1 < NT else None
                hT_cur = stage2(hf_cur)
                if hT_prev is not None:
                    stage3(ti - 1, hT_prev)
                hT_prev = hT_cur
                hf_cur = hf_next
            stage3(NT - 1, hT_prev)

    def phase_E(self, l):
        S = self.S
        NIT = 22
        with ExitStack() as es:
            Pp, TPp = self.sb(es, "PpE", [128, 4, NE, 8], F32)
            PT, TPT = self.sb(es, "PT", [128, 512], F32)
            mk, Tmk = self.sb(es, "mkE", [128, 512], F32)
            cum, Tcum = self.sb(es, "cumE", [128, 512], F32)
            ones, Tones = self.sb(es, "onesE", [128, 512], F32)
            jk, Tjk = self.sb(es, "jkE", [128, 512], F32)
            blk, Tblk = self.sb(es, "blkE", [128, 128], F32)
            ltri, Tltri = self.sb(es, "ltriE", [128, 128], F32)
            io16, Tio16 = self.sb(es, "io16E", [128, NE, 128], F32)
            tokf, Ttokf = self.sb(es, "tokfE", [128, NT, NE], F32)
            sdiv, Tsdiv = self.sb(es, "sdivE", [128, NT, NE], F32)
            smod, Tsmod = self.sb(es, "smodE", [128, NT, NE], F32)
            rhs8, Trhs8 = self.sb(es, "rhs8E", [128, NT, NE, 4, 2], F32)
            lo, Tlo = self.sb(es, "loE", [128, 1], F32)
            segt, Tsegt = self.sb(es, "segtE", [128, 2], F32)
            candR = self.ring(es, "candE", 2, [128, 1], F32)
            cntR = self.ring(es, "cntE", 2, [128, 2], F32)
            stpR = self.ring(es, "stpE", 2, [128, 1], F32)
            ohR = self.ring(es, "ohE", 3, [128, NE, 128], F32)
            ptp, Tptp = self.ps(es, "ptpE", [128, 512], F32)
            totR = self.ring(es, "totE", 2, [128, 2], F32, psum=True)
            pof, Tpof = self.ps(es, "pofE", [128, 2], F32)
            psl, Tpsl = self.ps(es, "pslE", [128, 4, NE, 8], F32)
            pig, Tpig = self.ps(es, "pigE", [128, NE, 4, 2], F32)

            self.dma("sp", blk[:, :], self.c_blk[:, :], [], [Tblk])
            self.dma("sp", ltri[:, :], self.c_ltri[:, :], [], [Tltri])
            self.dma("sp", io16[:, :, :], self.c_io16.rearrange("p (e b) -> p e b", b=128), [], [Tio16])
            self.dma("sp", tokf[:, :, :], self.c_tokf.rearrange("p (i e) -> p i e", e=NE), [], [Ttokf])
            self.memset("pool", ones[:, :], 1.0, [Tones])
            self.memset("dve", lo[:, :], 0.0, [Tlo])
            self.memset("dve", segt[:, :], 0.0, [Tsegt])
            for (c_, Tc_) in cntR.items:
                self.memset("dve", c_[:, :], 0.0, [Tc_])
            self.copy("dve", Pp[:, :, :, :], self.P_all[:, :, :].rearrange("p (s j) e -> p j e s", j=4),
                      [self.TP], [TPp])
            for jc in range(4):
                self.tr(ptp[:, jc * 128:(jc + 1) * 128], Pp[:, jc, :, :].rearrange("p e s -> p (e s)"),
                        self.identf[:, :], [TPp, self.Tidentf], [Tptp])
            self.copy("dve", PT[:, :], ptp[:, :], [Tptp], [TPT])
            step = 0.5
            for it in range(NIT):
                cand, Tcand = candR.next()
                cnt, Tcnt = cntR.next()
                stp, Tstp = stpR.next()
                tot, Ttot = totR.next()
                self.ts("dve", cand[:, 0:1], lo[:, 0:1], step, None, ALU.add, None, [Tlo], [Tcand])
                self.ts("dve", jk[:, :], PT[:, :], cand[:, 0:1], 0.0, ALU.is_ge, ALU.add, [TPT, Tcand], [Tjk, Tcnt],
                        accum_out=cnt[:, 0:1])
                self.mm(tot[:, 0:2], blk[:, :], cnt[:, 0:2], True, True, [Tblk, Tcnt], [Ttot])
                self.ts("dve", stp[:, 0:1], tot[:, 0:1], float(CAP) - 0.5, step, ALU.is_ge, ALU.mult, [Ttot], [Tstp])
                self.tt("dve", lo[:, 0:1], lo[:, 0:1], stp[:, 0:1], ALU.add, [Tstp, Tlo], [Tlo])
                step *= 0.5
            self.ts("dve", mk[:, :], PT[:, :], lo[:, 0:1], None, ALU.is_ge, None, [TPT, Tlo], [Tmk])
            S.add("dve", lambda e: e.tensor_tensor_scan(out=cum[:, :], data0=ones[:, :], data1=mk[:, :], initial=0.0,
                                                        op0=ALU.mult, op1=ALU.add), [Tones, Tmk], [Tcum])
            self.copy("dve", segt[:, 0:1], cum[:, 511:512], [Tcum], [Tsegt])
            self.mm(pof[:, 0:2], ltri[:, :], segt[:, 0:2], True, True, [Tltri, Tsegt], [Tpof])
            self.ts("dve", cum[:, :], cum[:, :], pof[:, 0:1], None, ALU.add, None, [Tcum, Tpof], [Tcum])
            self.tt("dve", cum[:, :], cum[:, :], mk[:, :], ALU.mult, [Tcum, Tmk], [Tcum])
            self.ts("dve", cum[:, :], cum[:, :], -1.0, None, ALU.add, None, [Tcum], [Tcum])
            for jc in range(4):
                self.tr(psl[:, jc, :, :].rearrange("p e s -> p (e s)"), cum[:, jc * 128:(jc + 1) * 128],
                        self.identf[:, :], [Tcum, self.Tidentf], [Tpsl])
            self.copy("dve", self.slot_all[:, :, :].rearrange("p (s j) e -> p j e s", j=4), psl[:, :, :, :],
                      [Tpsl], [self.Tslot])
            sl = self.slot_all
            self.ts("dve", sdiv[:, :, :], sl[:, :, :], 128.0, None, ALU.is_ge, None, [self.Tslot], [Tsdiv])
            self.stt(sdiv[:, :, :], sl[:, :, :], 256.0, sdiv[:, :, :], ALU.is_ge, ALU.add, [self.Tslot, Tsdiv], [Tsdiv])
            self.stt(sdiv[:, :, :], sl[:, :, :], 384.0, sdiv[:, :, :], ALU.is_ge, ALU.add, [self.Tslot, Tsdiv], [Tsdiv])
            self.stt(smod[:, :, :], sdiv[:, :, :], -128.0, sl[:, :, :], ALU.mult, ALU.add, [self.Tslot, Tsdiv], [Tsmod])
            for a_ in range(4):
                self.stt(rhs8[:, :, :, a_, 0], sdiv[:, :, :], float(a_), tokf[:, :, :], ALU.is_equal, ALU.mult,
                         [Tsdiv, Ttokf], [Trhs8])
                self.stt(rhs8[:, :, :, a_, 1], sdiv[:, :, :], float(a_), self.P_all[:, :, :], ALU.is_equal, ALU.mult,
                         [Tsdiv, self.TP], [Trhs8])
            for i in range(NT):
                oh, Toh = ohR.next()
                self.tt("dve", oh[:, :, :], io16[:, :, :], smod[:, i, :].unsqueeze(2).to_broadcast([128, NE, 128]),
                        ALU.is_equal, [Tio16, Tsmod], [Toh])
                for e_ in range(NE):
                    self.mm(pig[:, e_, :, :].rearrange("p a c -> p (a c)"), oh[:, e_, :],
                            rhs8[:, i, e_, :, :].rearrange("p a c -> p (a c)"),
                            i == 0 and e_ == 0, i == NT - 1 and e_ == NE - 1, [Toh, Trhs8], [Tpig])
            self.copy("dve", self.idx_all[:, :, :], pig[:, :, :, 0], [Tpig], [self.Tidx])
            self.copy("dve", self.gate_all[:, :, :], pig[:, :, :, 1], [Tpig], [self.Tgate])

    def phase_F(self, l):
        S = self.S
        with ExitStack() as es:
            wR = self.wR
            xs, Txs = self.xsF
            xsT, TxsT = self.sb(es, "xsTF", [128, 16, CAP], BF16)
            hid, Thid = self.sb(es, "hidF", [128, 16, CAP], BF16)
            sgR = self.ring(es, "sgF", 2, [128, CAP], F32)
            ysR = self.ring(es, "ysF", 4, [128, D], F32)
            ptR = self.ring(es, "ptF", 2, [128, 8, 128], BF16, psum=True)
            pgR = self.ring(es, "pgF", 2, [128, CAP], F32, psum=True)
            puR = self.ring(es, "puF", 2, [128, CAP], F32, psum=True)
            pdR = self.ring(es, "pdF", 2, [128, 512], F32, psum=True)
            Txres = S.tile("xres_scatter")

            def wload(src2d, blk):
                wt, Twt = wR.next()
                self.dma("pool", wt[:, :, :], src2d.rearrange("(c p) f -> p c f", p=128)[:, :, blk * 512:(blk + 1) * 512],
                         [], [Twt])
                return wt, Twt

            NPRE = 6

            def units_of(e_):
                units = []
                for fb in range(4):
                    units.append((self.w_gate[l, e_], fb))
                    units.append((self.w_up[l, e_], fb))
                for db in range(4):
                    units.append((self.w_down[l, e_], db))
                return units

            def gather(e_):
                for sc in range(4):
                    S.add("pool", lambda e, sc=sc, e_=e_: e.indirect_dma_start(
                        out=xs[:, sc, :], out_offset=None, in_=self.h2d[:, :],
                        in_offset=bass.IndirectOffsetOnAxis(ap=self.idx_all[:, e_, sc:sc + 1], axis=0)),
                        [self.Tidx], [Txs], dma=True)

            def transposes():
                for sc in range(4):
                    for half in range(2):
                        pt, Tpt = ptR.next()
                        for c in range(8):
                            cc = half * 8 + c
                            self.tr(pt[:, c, :], xs[:, sc, cc * 128:(cc + 1) * 128], self.identb[:, :],
                                    [Txs, self.Tidentb], [Tpt])
                        self.copy("act" if half == 0 else "dve", xsT[:, half * 8:(half + 1) * 8, sc * 128:(sc + 1) * 128],
                                  pt[:, :, :], [Tpt], [TxsT])

            def gate_up(units, loaded):
                nxt = NPRE
                for fb in range(4):
                    wg, Twg = loaded[2 * fb]
                    wu, Twu = loaded[2 * fb + 1]
                    for fcl in range(4):
                        fc = fb * 4 + fcl
                        pg, Tpg = pgR.next()
                        pu, Tpu = puR.next()
                        for dc in range(16):
                            self.mm(pg[:, :], wg[:, dc, fcl * 128:(fcl + 1) * 128], xsT[:, dc, :], dc == 0, dc == 15,
                                    [Twg, TxsT], [Tpg])
                        for dc in range(16):
                            self.mm(pu[:, :], wu[:, dc, fcl * 128:(fcl + 1) * 128], xsT[:, dc, :], dc == 0, dc == 15,
                                    [Twu, TxsT], [Tpu])
                        sg, Tsg = sgR.next()
                        self.act(sg[:, :], pg[:, :], AF.Silu, [Tpg], [Tsg])
                        self.tt("dve", hid[:, fc, :], sg[:, :], pu[:, :], ALU.mult, [Tsg, Tpu], [Thid])
                    for _ in range(2):
                        if nxt < len(units):
                            loaded.append(wload(*units[nxt]))
                            nxt += 1

            def down(e_, loaded, nu, nxt_loaded):
                ys_list = [ysR.next() for _ in range(4)]
                for db in range(4):
                    wd, Twd = loaded[8 + db]
                    for sc in range(4):
                        pd, Tpd = pdR.next()
                        for fc in range(16):
                            self.mm(pd[:, :], hid[:, fc, sc * 128:(sc + 1) * 128], wd[:, fc, :], fc == 0, fc == 15,
                                    [Thid, Twd], [Tpd])
                        ys, Tys = ys_list[sc]
                        if (db * 4 + sc) % 2 == 0:
                            self.ts("dve", ys[:, db * 512:(db + 1) * 512], pd[:, :], self.gate_all[:, e_, sc:sc + 1],
                                    None, ALU.mult, None, [Tpd, self.Tgate], [Tys])
                        else:
                            self.act(ys[:, db * 512:(db + 1) * 512], pd[:, :], AF.Copy, [Tpd, self.Tgate], [Tys],
                                     scale=self.gate_all[:, e_, sc:sc + 1])
                    if nu is not None and len(nxt_loaded) < NPRE:
                        nxt_loaded.append(wload(*nu[len(nxt_loaded)]))

                def scatter():
                    for sc in range(4):
                        ys, Tys = ys_list[sc]
                        S.add("pool", lambda e, sc=sc, ys=ys: e.indirect_dma_start(
                            out=self.xres[:, :],
                            out_offset=bass.IndirectOffsetOnAxis(ap=self.idx_all[:, e_, sc:sc + 1], axis=0),
                            in_=ys[:, :], in_offset=None, compute_op=ALU.add),
                            [Tys, self.Tidx], [Txres], dma=True)
                return scatter

            gather(0)
            transposes()
            gather(1)
            loaded = list(self.pref)
            pending_scatter = None
            for e_ in range(NE):
                units = units_of(e_)
                gate_up(units, loaded)
                nxt_loaded = None
                if pending_scatter is not None:
                    pending_scatter()
                    pending_scatter = None
                nu = None
                if e_ + 1 < NE:
                    nu = units_of(e_ + 1)
                    nxt_loaded = [wload(*nu[ui]) for ui in range(3)]
                    transposes()
                    if e_ + 2 < NE:
                        gather(e_ + 2)
                pending_scatter = down(e_, loaded, nu, nxt_loaded)
                assert nu is None or len(nxt_loaded) == NPRE
                loaded = nxt_loaded
            pending_scatter()

    def phase_G(self):
        with ExitStack() as es:
            gb, Tgb = self.sb(es, "gbG", [128, D], F32)
            junk, Tjunk = self.sb(es, "junkG", [128, D], BF16)
            xtR = self.ring(es, "xtG", 3, [128, D], F32)
            oR = self.ring(es, "oG", 3, [128, D], F32)
            small = self.ring(es, "smG", 12, [128, 1], F32)
            self.dma("sp", gb[:, :], self.final_g[0].partition_broadcast(128), [], [Tgb])
            for ti in range(NT):
                t0 = ti * 128
                xt, Txt = xtR.next()
                self.dma("sp", xt[:, :], self.xres[t0:t0 + 128, :], [], [Txt])
                rs, Trs = self.rstd_of(xt, Txt, junk, Tjunk, small)
                o, To = oR.next()
                self.stt(o[:, :], xt[:, :], rs[:, 0:1], gb[:, :], ALU.mult, ALU.mult, [Txt, Trs, Tgb], [To])
                self.dma("sp", self.out[t0:t0 + 128, :], o[:, :], [To], [])


_CONST = {}


def _constants():
    if _CONST:
        return _CONST
    bf = ml_dtypes.bfloat16
    c = {}
    c["c_identb"] = np.eye(128, dtype=np.float32).astype(bf)
    c["c_identf"] = np.eye(128, dtype=np.float32)
    n = np.arange(256)
    ang = 2.0 * np.pi * ((n[:, None] * n[None, :]) % 256) / 256.0
    sc = 1.0 / 1024.0
    c["c_cs"] = np.concatenate([np.cos(ang) * sc, -np.sin(ang) * sc], axis=1).astype(np.float32).astype(bf)
    t = np.arange(L, dtype=np.int64)
    tk = (t[:, None] * t[None, :]) % L
    ang = (2.0 * np.pi / L) * tk
    cm = np.cos(ang).astype(np.float32)
    sm = np.sin(ang).astype(np.float32)
    def tile_dft(m):
        m4 = np.ascontiguousarray(m[:, :L // 2]).reshape(32, 128, 8, 256)
        return np.ascontiguousarray(m4.transpose(2, 1, 0, 3)).reshape(8, 128, 32 * 256).astype(bf)
    c["c_dftc"] = tile_dft(cm)
    c["c_dfts"] = tile_dft(sm)
    sgn = np.where((np.arange(L) % 2) == 0, 1.0, -1.0).astype(np.float32).reshape(32, 128).T
    c["c_nyq"] = np.ascontiguousarray(np.repeat(sgn[:, :, None], 2, axis=2)).reshape(128, 64).astype(bf)
    inv = np.zeros((4, 16), np.float32)
    for gi, w in enumerate((2, 4, 8, 16)):
        for j in range(8):
            tt = j
            lo = max(tt - w // 2, 0); hi = min(tt + w - w // 2, L)
            inv[gi, j] = 1.0 / (hi - lo)
            tt = L - 8 + j
            lo = max(tt - w // 2, 0); hi = min(tt + w - w // 2, L)
            inv[gi, 8 + j] = 1.0 / (hi - lo)
    c["c_invedge"] = inv.reshape(1, 64)
    q = np.arange(128)
    c["c_blk"] = (q[:, None] // 8 == q[None, :] // 8).astype(np.float32)
    c["c_ltri"] = ((q[:, None] // 8 == q[None, :] // 8) & (q[:, None] % 8 < q[None, :] % 8)).astype(np.float32)
    c["c_io16"] = np.tile(np.arange(128, dtype=np.float32)[None, :], (128, NE))
    tok = (np.arange(NT, dtype=np.float32)[None, :] * 128 + np.arange(128, dtype=np.float32)[:, None])
    c["c_tokf"] = np.ascontiguousarray(np.repeat(tok[:, :, None], NE, axis=2)).reshape(128, NT * NE)
    _CONST.update(c)
    return _CONST


_NC = {}


def _get_nc():
    if "nc" not in _NC:
        _NC["nc"] = Builder().build()
    return _NC["nc"]


def kernel(x, norm1_g, w_in, w_fourier, w_pool, pool_scale, w_out, norm2_g, w_router, w_gate, w_up, w_down,
           final_g):
    f = lambda a: np.ascontiguousarray(np.asarray(a, dtype=np.float32))
    shared = {
        "norm1_g": f(norm1_g), "w_in": f(w_in), "w_fourier": f(w_fourier), "w_pool": f(w_pool),
        "pool_scale": f(pool_scale), "w_out": f(w_out), "norm2_g": f(norm2_g), "w_router": f(w_router),
        "w_gate": f(w_gate), "w_up": f(w_up), "w_down": f(w_down), "final_g": f(final_g).reshape(1, D),
    }
    shared.update(_constants())
    x = f(x)
    nc = _get_nc()
    in_maps = []
    for c in range(NCORES):
        m = dict(shared)
        m["x"] = x[c]
        in_maps.append(m)
    res = run_bass_kernel_spmd(nc, in_maps, core_ids=list(range(NCORES)))
    return np.stack([res.results[c]["out"] for c in range(NCORES)], axis=0).astype(np.float32)
```

```python
import numpy as np
import ml_dtypes
from contextlib import ExitStack
import concourse.bass as bass
import concourse.mybir as mybir
from concourse.bass_utils import run_bass_kernel_spmd

F32 = mybir.dt.float32
BF16 = mybir.dt.bfloat16
U32 = mybir.dt.uint32
ALU = mybir.AluOpType
AF = mybir.ActivationFunctionType
AX = mybir.AxisListType

L = 4096
D = 2048
NL = 2
NE = 16
CAP = 512
NT = L // 128
EPS = 1e-6
NCORES = 4


class Tl:
    __slots__ = ("name", "w", "r", "rd")

    def __init__(self, name):
        self.name = name
        self.w = None
        self.r = {}
        self.rd = []


class Op:
    __slots__ = ("eng", "fn", "dma", "waits", "idx", "needs_inc", "sem", "val", "prev", "incval")


class Sched:
    ENGS = ("pe", "act", "dve", "pool", "sp")

    def __init__(self, nc, csem, pools):
        self.nc = nc
        self.csem = csem
        self.pools = pools
        self.ops = {e: [] for e in self.ENGS}
        self.waited = {e: {} for e in self.ENGS}
        self.dma_waited = {e: set() for e in self.ENGS}
        self.dma_count = {e: 0 for e in self.ENGS}
        self.last_compute = {e: None for e in self.ENGS}
        self.outstanding = []
        self.tiles = []

    def tile(self, name):
        t = Tl(name)
        self.tiles.append(t)
        return t

    def add(self, eng, fn, reads=(), writes=(), dma=False):
        op = Op()
        op.eng = eng
        op.fn = fn
        op.dma = dma
        op.idx = len(self.ops[eng])
        op.needs_inc = False
        op.sem = None
        op.val = 0
        op.prev = None
        op.incval = 0
        deps = []
        for t in reads:
            if t.w is not None:
                deps.append(t.w)
        for t in writes:
            if t.w is not None:
                deps.append(t.w)
            deps.extend(t.r.values())
            deps.extend(t.rd)
        waits = []
        for d in deps:
            if d is op:
                continue
            if d.dma:
                if d in self.dma_waited[eng]:
                    continue
                self.dma_waited[eng].add(d)
                waits.append(d)
            else:
                if eng == "pe" and d.eng == "pe" and not dma:
                    continue
                if self.waited[eng].get(d.eng, -1) >= d.idx:
                    continue
                self.waited[eng][d.eng] = d.idx
                d.needs_inc = True
                waits.append(d)
        if dma:
            pool = self.pools[eng]
            k = self.dma_count[eng]
            op.sem = pool[k % len(pool)]
            uses = k // len(pool)
            op.val = 16 * (uses + 1)
            op.prev = (op.sem, 16 * uses) if uses > 0 else None
            self.dma_count[eng] += 1
            self.outstanding.append(op)
        else:
            self.last_compute[eng] = op
        op.waits = waits
        for t in reads:
            if dma:
                t.rd.append(op)
            else:
                t.r[eng] = op
        for t in writes:
            t.w = op
            t.r = {}
            t.rd = []
        self.ops[eng].append(op)
        return op

    def barrier(self):
        best = {}
        for d in self.outstanding:
            key = id(d.sem)
            if key not in best or best[key].val < d.val:
                best[key] = d
        for eng in self.ENGS:
            waits = []
            for e2 in self.ENGS:
                j = self.last_compute[e2]
                if j is None:
                    continue
                if self.waited[eng].get(e2, -1) >= j.idx:
                    continue
                self.waited[eng][e2] = j.idx
                j.needs_inc = True
                waits.append(j)
            for d in best.values():
                waits.append(d)
            op = Op()
            op.eng = eng
            op.fn = None
            op.dma = False
            op.idx = len(self.ops[eng])
            op.needs_inc = False
            op.sem = None
            op.val = 0
            op.prev = None
            op.incval = 0
            op.waits = waits
            self.ops[eng].append(op)
            self.dma_waited[eng] = set()
        self.outstanding = []
        for t in self.tiles:
            t.w = None
            t.r = {}
            t.rd = []

    def emit(self, block):
        for eng in self.ENGS:
            c = 0
            for op in self.ops[eng]:
                if op.needs_inc:
                    c += 1
                    op.incval = c

        def run(eng, e):
            for op in self.ops[eng]:
                w = {}
                if op.dma and op.prev is not None:
                    w[id(op.prev[0])] = (op.prev[0], op.prev[1])
                for d in op.waits:
                    if d.dma:
                        s, v = d.sem, d.val
                    else:
                        s, v = self.csem[d.eng], d.incval
                    if id(s) not in w or w[id(s)][1] < v:
                        w[id(s)] = (s, v)
                for s, v in w.values():
                    e.wait_ge(s, v)
                if op.fn is not None:
                    ins = op.fn(e)
                    if op.dma:
                        ins.then_inc(op.sem, 16)
                    elif op.needs_inc:
                        ins.then_inc(self.csem[eng], 1)

        block.tensor(lambda e: run("pe", e))
        block.scalar(lambda e: run("act", e))
        block.vector(lambda e: run("dve", e))
        block.gpsimd(lambda e: run("pool", e))
        block.sync(lambda e: run("sp", e))


class Ring:
    def __init__(self, items):
        self.items = items
        self.k = 0

    def next(self):
        it = self.items[self.k % len(self.items)]
        self.k += 1
        return it


class Builder:
    def __init__(self, nlayers=NL, stop_after=None, debug=False, unit=None):
        self.unit = unit
        self.nlayers = nlayers
        self.stop_after = stop_after
        self.debug = debug

    def sb(self, es, name, shape, dt):
        self.uid = getattr(self, "uid", 0) + 1
        name = "%s_%d" % (name, self.uid)
        h = es.enter_context(self.nc.sbuf_tensor(name, shape, dt))
        return h, self.S.tile(name)

    def ps(self, es, name, shape, dt):
        self.uid = getattr(self, "uid", 0) + 1
        name = "%s_%d" % (name, self.uid)
        h = es.enter_context(self.nc.psum_tensor(name, shape, dt))
        return h, self.S.tile(name)

    def ring(self, es, name, n, shape, dt, psum=False):
        items = []
        for i in range(n):
            items.append((self.ps if psum else self.sb)(es, "%s%d" % (name, i), shape, dt))
        return Ring(items)

    def dma(self, eng, out, in_, reads, writes, **kw):
        return self.S.add(eng, lambda e: e.dma_start(out=out, in_=in_, **kw), reads, writes, dma=True)

    def mm(self, out, lhsT, rhs, start, stop, reads, writes):
        return self.S.add("pe", lambda e: e.matmul(out, lhsT, rhs, start=start, stop=stop), reads, writes)

    def tr(self, out, in_, ident, reads, writes):
        return self.S.add("pe", lambda e: e.transpose(out, in_, ident), reads, writes)

    def act(self, out, in_, func, reads, writes, **kw):
        return self.S.add("act", lambda e: e.activation(out=out, in_=in_, func=func, **kw), reads, writes)

    def copy(self, eng, out, in_, reads, writes):
        if eng == "act":
            return self.S.add("act", lambda e: e.copy(out=out, in_=in_), reads, writes)
        return self.S.add(eng, lambda e: e.tensor_copy(out=out, in_=in_), reads, writes)

    def tt(self, eng, out, in0, in1, op, reads, writes):
        return self.S.add(eng, lambda e: e.tensor_tensor(out=out, in0=in0, in1=in1, op=op), reads, writes)

    def ts(self, eng, out, in0, s1, s2, op0, op1, reads, writes, accum_out=None):
        if op1 is None:
            return self.S.add(eng, lambda e: e.tensor_scalar(out=out, in0=in0, scalar1=s1, scalar2=None, op0=op0),
                              reads, writes)
        if accum_out is not None:
            return self.S.add(eng, lambda e: e.tensor_scalar(out=out, in0=in0, scalar1=s1, scalar2=s2, op0=op0,
                                                             op1=op1, accum_out=accum_out), reads, writes)
        return self.S.add(eng, lambda e: e.tensor_scalar(out=out, in0=in0, scalar1=s1, scalar2=s2, op0=op0, op1=op1),
                          reads, writes)

    def stt(self, out, in0, scalar, in1, op0, op1, reads, writes):
        return self.S.add("dve", lambda e: e.scalar_tensor_tensor(out=out, in0=in0, scalar=scalar, in1=in1,
                                                                  op0=op0, op1=op1), reads, writes)

    def memset(self, eng, ap, val, writes):
        return self.S.add(eng, lambda e: e.memset(ap, val), (), writes)

    def rstd_of(self, xt, Txt, junk, Tjunk, small):
        ss, Tss = small.next()
        sd, Tsd = small.next()
        rs, Trs = small.next()
        self.act(junk[:, :], xt[:, :], AF.Square, [Txt], [Tjunk, Tss], accum_out=ss[:, 0:1])
        self.act(sd[:, 0:1], ss[:, 0:1], AF.Sqrt, [Tss, self.Teps], [Tsd], scale=1.0 / D, bias=self.eps[:, 0:1])
        self.S.add("dve", lambda e: e.reciprocal(out=rs[:, 0:1], in_=sd[:, 0:1]), [Tsd], [Trs])
        return rs, Trs

    def build(self):
        nc = bass.Bass("TRN2", target_bir_lowering=False)
        self.nc = nc
        def dt_in(name, shape, dt=F32):
            if self.unit and not name.startswith("c_"):
                return None
            return nc.dram_tensor(name, shape, dt, kind="ExternalInput").ap()
        self.x = dt_in("x", [L, D])
        self.norm1_g = dt_in("norm1_g", [NL, D])
        self.w_in = dt_in("w_in", [NL, D, D])
        self.w_fourier = dt_in("w_fourier", [NL, 4, 256, 256])
        self.w_pool = dt_in("w_pool", [NL, 4, 256, 256])
        self.pool_scale = dt_in("pool_scale", [NL, 4, 256])
        self.w_out = dt_in("w_out", [NL, D, D])
        self.norm2_g = dt_in("norm2_g", [NL, D])
        self.w_router = dt_in("w_router", [NL, D, NE])
        self.w_gate = dt_in("w_gate", [NL, NE, D, D])
        self.w_up = dt_in("w_up", [NL, NE, D, D])
        self.w_down = dt_in("w_down", [NL, NE, D, D])
        self.final_g = dt_in("final_g", [1, D])
        self.c_identb = dt_in("c_identb", [128, 128], BF16)
        self.c_identf = dt_in("c_identf", [128, 128])
        self.c_cs = dt_in("c_cs", [256, 512], BF16)
        self.c_dftc = dt_in("c_dftc", [8, 128, 32 * 256], BF16)
        self.c_dfts = dt_in("c_dfts", [8, 128, 32 * 256], BF16)
        self.c_nyq = dt_in("c_nyq", [128, 32 * 2], BF16)
        self.c_invedge = dt_in("c_invedge", [1, 4 * 16])
        self.c_blk = dt_in("c_blk", [128, 128])
        self.c_ltri = dt_in("c_ltri", [128, 128])
        self.c_io16 = dt_in("c_io16", [128, NE * 128])
        self.c_tokf = dt_in("c_tokf", [128, NT * NE])
        self.out = nc.dram_tensor("out", [L, D] if not self.unit else [128, 8], F32, kind="ExternalOutput").ap()
        if self.unit:
            self.u_p = nc.dram_tensor("u_p", [128, NT * NE], F32, kind="ExternalInput").ap()
        self.xres = nc.dram_tensor("xres", [L, D], F32).ap()
        self.Gd = nc.dram_tensor("Gd", [4, L, 512], BF16).ap()
        self.uBd = nc.dram_tensor("uBd", [1024, L], F32).ap()
        self.yT = nc.dram_tensor("yT", [D, L], BF16).ap()
        self.h2d = nc.dram_tensor("h2d", [L, D], BF16).ap()
        if self.debug:
            self.dbg_yT = nc.dram_tensor("dbg_yT", [D, L], BF16, kind="ExternalOutput").ap()
            self.dbg_x = nc.dram_tensor("dbg_x", [L, D], F32, kind="ExternalOutput").ap()
            self.dbg_p = nc.dram_tensor("dbg_p", [128, NT * NE], F32, kind="ExternalOutput").ap()
            self.dbg_idx = nc.dram_tensor("dbg_idx", [128, NE * 4], U32, kind="ExternalOutput").ap()
            self.dbg_gate = nc.dram_tensor("dbg_gate", [128, NE * 4], F32, kind="ExternalOutput").ap()

        with ExitStack() as top:
            csem = {}
            for e in Sched.ENGS[:4]:
                csem[e] = top.enter_context(nc.semaphore("c_" + e))
            pools = {}
            for e, n in (("sp", 40), ("pool", 24), ("act", 12), ("dve", 12)):
                pools[e] = [top.enter_context(nc.semaphore("d_%s%d" % (e, i))) for i in range(n)]
            self.S = Sched(nc, csem, pools)
            block = top.enter_context(nc.Block())
            with ExitStack() as es:
                self.identb, self.Tidentb = self.sb(es, "identb", [128, 128], BF16)
                self.identf, self.Tidentf = self.sb(es, "identf", [128, 128], F32)
                self.eps, self.Teps = self.sb(es, "eps", [128, 1], F32)
                self.dma("sp", self.identb[:, :], self.c_identb[:, :], [], [self.Tidentb])
                self.dma("sp", self.identf[:, :], self.c_identf[:, :], [], [self.Tidentf])
                self.memset("dve", self.eps[:, :], EPS, [self.Teps])
                if self.unit == "E":
                    self.unit_E(es)
                else:
                    self.body(es)
                self.S.barrier()
            self.S.emit(block)
        return nc

    def unit_E(self, es0):
        with ExitStack() as es:
            self.P_all, self.TP = self.sb(es, "P_all", [128, NT, NE], F32)
            self.slot_all, self.Tslot = self.sb(es, "slot_all", [128, NT, NE], F32)
            self.idx_all, self.Tidx = self.sb(es, "idx_all", [128, NE, 4], U32)
            self.gate_all, self.Tgate = self.sb(es, "gate_all", [128, NE, 4], F32)
            self.dma("sp", self.P_all[:, :, :], self.u_p.rearrange("p (i e) -> p i e", e=NE), [], [self.TP])
            self.phase_E(0)
            self.S.barrier()
            self.dma("sp", self.dbg_idx[:, :], self.idx_all[:, :, :], [self.Tidx], [])
            self.dma("sp", self.dbg_gate[:, :], self.gate_all[:, :, :], [self.Tgate], [])

    def body(self, es0):
        S = self.S
        for l in range(self.nlayers):
            xsrc = self.x if l == 0 else self.xres
            self.phase_A(l, xsrc)
            S.barrier()
            if self.stop_after == ("A", l):
                return
            self.phase_BC(l)
            S.barrier()
            if self.stop_after == ("C", l):
                self.dma("sp", self.dbg_yT[:, :], self.yT[:, :], [], [])
                return
            with ExitStack() as es:
                self.P_all, self.TP = self.sb(es, "P_all", [128, NT, NE], F32)
                self.slot_all, self.Tslot = self.sb(es, "slot_all", [128, NT, NE], F32)
                self.idx_all, self.Tidx = self.sb(es, "idx_all", [128, NE, 4], U32)
                self.gate_all, self.Tgate = self.sb(es, "gate_all", [128, NE, 4], F32)
                self.phase_D(l, xsrc)
                S.barrier()
                if self.stop_after == ("D", l):
                    self.dma("sp", self.dbg_x[:, :], self.xres[:, :], [], [])
                    self.dma("sp", self.dbg_p[:, :], self.P_all[:, :, :], [self.TP], [])
                    return
                esw = es.enter_context(ExitStack())
                self.wR = self.ring(esw, "wF", 7, [128, 16, 512], BF16)
                self.xsF = self.sb(esw, "xsF", [128, 4, D], BF16)
                self.pref = []
                units0 = [(self.w_gate[l, 0], 0), (self.w_up[l, 0], 0), (self.w_gate[l, 0], 1), (self.w_up[l, 0], 1),
                          (self.w_gate[l, 0], 2), (self.w_up[l, 0], 2)]
                for (src2d, blk) in units0:
                    wt, Twt = self.wR.next()
                    self.dma("pool", wt[:, :, :],
                             src2d.rearrange("(c p) f -> p c f", p=128)[:, :, blk * 512:(blk + 1) * 512], [], [Twt])
                    self.pref.append((wt, Twt))
                self.phase_E(l)
                S.barrier()
                if self.stop_after == ("E", l):
                    self.dma("sp", self.dbg_x[:, :], self.xres[:, :], [], [])
                    self.dma("sp", self.dbg_p[:, :], self.P_all[:, :, :], [self.TP], [])
                    self.dma("sp", self.dbg_idx[:, :], self.idx_all[:, :, :], [self.Tidx], [])
                    self.dma("sp", self.dbg_gate[:, :], self.gate_all[:, :, :], [self.Tgate], [])
                    return
                self.phase_F(l)
                S.barrier()
            if self.stop_after == ("F", l):
                self.dma("sp", self.dbg_x[:, :], self.xres[:, :], [], [])
                return
        self.phase_G()

    def phase_A(self, l, xsrc):
        S = self.S
        with ExitStack() as es:
            win, _ = self.sb(es, "win", [128, 16, D], BF16)
            Twin = [S.tile("winq%d" % q) for q in range(4)]
            gb, Tgb = self.sb(es, "gb", [128, D], F32)
            cs, Tcs = self.sb(es, "cs", [128, 2, 512], BF16)
            junk, Tjunk = self.sb(es, "junkA", [128, D], BF16)
            xtR = self.ring(es, "xtA", 3, [128, D], F32)
            hbR = self.ring(es, "hbA", 8, [128, D], BF16)
            hTR = self.ring(es, "hTA", 2, [128, 16, 512], BF16)
            uTfR = self.ring(es, "uTfA", 2, [128, 8, 512], BF16)
            upR = self.ring(es, "upA", 4, [128, 512], F32)
            gtR = self.ring(es, "gtA", 4, [128, 512], BF16)
            small = self.ring(es, "smA", 24, [128, 1], F32)
            ptR = self.ring(es, "ptA", 3, [128, 8, 128], BF16, psum=True)
            puR = self.ring(es, "puA", 3, [128, 512], F32, psum=True)
            pgR = self.ring(es, "pgA", 2, [128, 512], F32, psum=True)

            wsrc = self.w_in[l].rearrange("(c p) f -> p c f", p=128)
            for q in range(4):
                self.dma("pool", win[:, :, q * 512:(q + 1) * 512], wsrc[:, :, q * 512:(q + 1) * 512], [], [Twin[q]])
            self.dma("sp", gb[:, :], self.norm1_g[l].partition_broadcast(128), [], [Tgb])
            self.dma("sp", cs[:, :, :], self.c_cs.rearrange("(c p) f -> p c f", p=128), [], [Tcs])

            def norm(tb):
                hbs = []
                for i in range(4):
                    t0 = tb * 512 + i * 128
                    xt, Txt = xtR.next()
                    self.dma("sp", xt[:, :], xsrc[t0:t0 + 128, :], [], [Txt])
                    rs, Trs = self.rstd_of(xt, Txt, junk, Tjunk, small)
                    hb, Thb = hbR.next()
                    self.stt(hb[:, :], xt[:, :], rs[:, 0:1], gb[:, :], ALU.mult, ALU.mult, [Txt, Trs, Tgb], [Thb])
                    hbs.append((hb, Thb))
                return hbs

            def transp(hbs):
                hT, ThT = hTR.next()
                for i in range(4):
                    hb, Thb = hbs[i]
                    for half in range(2):
                        pt, Tpt = ptR.next()
                        for c in range(8):
                            cc = half * 8 + c
                            self.tr(pt[:, c, :], hb[:, cc * 128:(cc + 1) * 128], self.identb[:, :],
                                    [Thb, self.Tidentb], [Tpt])
                        self.copy("act" if half == 0 else "dve", hT[:, half * 8:(half + 1) * 8, i * 128:(i + 1) * 128],
                                  pt[:, :, :], [Tpt], [ThT])
                return hT, ThT

            self.evA = 0

            def mms(tb, hT, ThT):
                uTf, TuTf = uTfR.next()
                for cchunk in range(16):
                    pu, Tpu = puR.next()
                    for dc in range(16):
                        self.mm(pu[:, :], win[:, dc, cchunk * 128:(cchunk + 1) * 128], hT[:, dc, :],
                                dc == 0, dc == 15, [Twin[cchunk // 4], ThT], [Tpu])
                    if cchunk < 8:
                        self.copy("act" if cchunk % 2 == 0 else "dve", uTf[:, cchunk, :], pu[:, :], [Tpu], [TuTf])
                    else:
                        up, Tup = upR.next()
                        self.copy("act" if cchunk % 2 == 0 else "dve", up[:, :], pu[:, :], [Tpu], [Tup])
                        r0 = (cchunk - 8) * 128
                        self.dma("pool", self.uBd[r0:r0 + 128, tb * 512:(tb + 1) * 512], up[:, :], [Tup], [])
                for i in range(4):
                    for g in range(4):
                        pg, Tpg = pgR.next()
                        for cc in range(2):
                            self.mm(pg[:, :], uTf[:, 2 * g + cc, i * 128:(i + 1) * 128], cs[:, cc, :],
                                    cc == 0, cc == 1, [TuTf, Tcs], [Tpg])
                        gt, Tgt = gtR.next()
                        self.copy("act" if self.evA % 2 == 0 else "dve", gt[:, :], pg[:, :], [Tpg], [Tgt])
                        self.evA += 1
                        t0 = tb * 512 + i * 128
                        self.dma("pool", self.Gd[g, t0:t0 + 128, :], gt[:, :], [Tgt], [])

            hb0 = norm(0)
            hb1 = norm(1)
            cur = transp(hb0)
            nxt_hb = hb1
            for tb in range(8):
                hb2 = norm(tb + 2) if tb + 2 < 8 else None
                nxt = transp(nxt_hb) if tb + 1 < 8 else None
                mms(tb, cur[0], cur[1])
                cur = nxt
                nxt_hb = hb2

    def phase_BC(self, l):
        W = L + 32
        with ExitStack() as es:
            GsR = self.ring(es, "GsB", 1, [128, 32, 512], BF16)
            CbR = self.ring(es, "CbB", 2, [128, 32, 256], BF16)
            SbR = self.ring(es, "SbB", 2, [128, 32, 256], BF16)
            nyq, Tnyq = self.sb(es, "nyqB", [128, 32, 2], BF16)
            aR = self.ring(es, "aB", 2, [128, 256], F32)
            fpR = self.ring(es, "fpB", 2, [128, 2, 256], BF16)
            fmR = self.ring(es, "fmB", 2, [128, 2, 256], BF16)
            fan, Tfan = self.sb(es, "fanB", [128, 2, 2], BF16)
            ystR = self.ring(es, "ystB", 1, [128, 2, L], BF16)
            wfR = self.ring(es, "wfB", 2, [128, 2, 256], BF16)
            upR = self.ring(es, "upC", 1, [128, W], F32)
            ra, Tra = self.sb(es, "raC", [128, W], F32)
            rb, Trb = self.sb(es, "rbC", [128, W], F32)
            plR = self.ring(es, "plC", 1, [128, 2, L], BF16)
            ybR = self.ring(es, "ybC", 1, [128, 2, L], BF16)
            wpR = self.ring(es, "wpC", 2, [128, 2, 256], BF16)
            psc, Tpsc = self.sb(es, "pscC", [128, 4, 2], F32)
            ied, Tied = self.sb(es, "iedC", [128, 4, 16], F32)
            e1, Te1 = self.sb(es, "e1C", [128, 16], F32)
            paR = self.ring(es, "paB", 2, [128, 256], F32, psum=True)
            pbR = self.ring(es, "pbB", 2, [128, 256], F32, psum=True)
            pyR = self.ring(es, "pyB", 2, [128, 256], F32, psum=True)
            ppR = self.ring(es, "ppC", 2, [128, 512], F32, psum=True)

            for (u_, Tu_) in upR.items:
                self.memset("pool", u_[:, :], 0.0, [Tu_])
            self.memset("dve", ra[:, :], 0.0, [Tra])
            self.memset("dve", rb[:, :], 0.0, [Trb])
            self.dma("sp", nyq[:, :, :], self.c_nyq.rearrange("p (i k) -> p i k", k=2), [], [Tnyq])
            self.dma("sp", psc[:, :, :], self.pool_scale[l].rearrange("g (q p) -> p g q", p=128), [], [Tpsc],
                     allow_slow_non_contiguous=True)
            self.dma("sp", ied[:, :, :], self.c_invedge[0].partition_broadcast(128).rearrange("p (g k) -> p g k", k=16),
                     [], [Tied])

            def pool_ops(g, pl, Tpl):
                w = (2, 4, 8, 16)[g]
                ops = []
                lo, hi = 8, W - 8
                for cc in range(2):
                    up, Tup = upR.next()
                    r0 = (g * 2 + cc) * 128
                    ops.append(lambda up=up, Tup=Tup, r0=r0: self.dma("sp", up[:, 16:16 + L], self.uBd[r0:r0 + 128, :],
                                                                      [], [Tup]))
                    ops.append(lambda up=up, Tup=Tup: self.tt("dve", ra[:, lo:hi], up[:, lo - 1:hi - 1], up[:, lo:hi],
                                                              ALU.add, [Tup], [Tra]))
                    cur, Tcur, oth, Toth = ra, Tra, rb, Trb
                    sh = 1
                    for lev in range(g):
                        ops.append(lambda cur=cur, Tcur=Tcur, oth=oth, Toth=Toth, sh=sh: self.tt(
                            "dve", oth[:, lo:hi], cur[:, lo - sh:hi - sh], cur[:, lo + sh:hi + sh], ALU.add,
                            [Tcur], [Toth]))
                        cur, Tcur, oth, Toth = oth, Toth, cur, Tcur
                        sh *= 2
                    ops.append(lambda cur=cur, Tcur=Tcur, up=up, Tup=Tup, cc=cc: self.stt(
                        pl[:, cc, :], cur[:, 16:16 + L], 1.0 / w, up[:, 16:16 + L], ALU.mult, ALU.subtract,
                        [Tcur, Tup], [Tpl]))

                    def edges(cur=cur, Tcur=Tcur, up=up, Tup=Tup, cc=cc):
                        self.tt("dve", e1[:, 0:8], cur[:, 16:24], ied[:, g, 0:8], ALU.mult, [Tcur, Tied], [Te1])
                        self.tt("dve", e1[:, 8:16], cur[:, 16 + L - 8:16 + L], ied[:, g, 8:16], ALU.mult,
                                [Tcur, Tied], [Te1])
                        self.tt("dve", pl[:, cc, 0:8], e1[:, 0:8], up[:, 16:24], ALU.subtract, [Te1, Tup], [Tpl])
                        self.tt("dve", pl[:, cc, L - 8:L], e1[:, 8:16], up[:, 16 + L - 8:16 + L], ALU.subtract,
                                [Te1, Tup], [Tpl])
                    ops.append(edges)
                return ops

            def ystage(kb, fp, Tfp, fm, Tfm, wf, Twf, yst, Tyst):
                j0 = 1 if kb == 0 else 0
                mstart = L - kb * 256 - j0
                n = 256 - j0
                for dq in range(2):
                    py, Tpy = pyR.next()
                    for cq in range(2):
                        self.mm(py[:, :], wf[:, cq, dq * 128:(dq + 1) * 128], fp[:, cq, :], cq == 0, cq == 1,
                                [Twf, Tfp], [Tpy])
                    self.copy("act", yst[:, dq, kb * 256:(kb + 1) * 256], py[:, :], [Tpy], [Tyst])
                    py, Tpy = pyR.next()
                    for cq in range(2):
                        self.mm(py[:, :], wf[:, cq, dq * 128:(dq + 1) * 128], fm[:, cq, :], cq == 0, cq == 1,
                                [Twf, Tfm], [Tpy])
                    self.copy("act", yst[:, dq, mstart:mstart - n:-1], py[:, j0:256], [Tpy], [Tyst])

            pend = None
            for g in range(4):
                Gs, TGs = GsR.next()
                self.dma("sp", Gs[:, :, :], self.Gd[g].rearrange("(i p) f -> p i f", p=128), [], [TGs])
                wf, Twf = wfR.next()
                self.dma("pool", wf[:, :, :], self.w_fourier[l, g].rearrange("(c p) d -> p c d", p=128), [], [Twf])
                wp, Twp = wpR.next()
                self.dma("pool", wp[:, :, :], self.w_pool[l, g].rearrange("(c p) d -> p c d", p=128), [], [Twp])
                yst, Tyst = ystR.next()
                pl, Tpl = plR.next()
                cops = pool_ops(g, pl, Tpl)
                per_kb = (len(cops) + 7) // 8
                for kb in range(8):
                    Cb, TCb = CbR.next()
                    Sb, TSb = SbR.next()
                    self.dma("sp", Cb[:, :, :], self.c_dftc[kb].rearrange("p (i k) -> p i k", k=256), [], [TCb])
                    self.dma("sp", Sb[:, :, :], self.c_dfts[kb].rearrange("p (i k) -> p i k", k=256), [], [TSb])
                    fp, Tfp = fpR.next()
                    fm, Tfm = fmR.next()
                    for cq in range(2):
                        pa, Tpa = paR.next()
                        pb, Tpb = pbR.next()
                        for i in range(32):
                            self.mm(pa[:, :], Gs[:, i, cq * 128:(cq + 1) * 128], Cb[:, i, :], i == 0, i == 31,
                                    [TGs, TCb], [Tpa])
                        for i in range(32):
                            self.mm(pb[:, :], Gs[:, i, 256 + cq * 128:256 + (cq + 1) * 128], Sb[:, i, :], i == 0,
                                    i == 31, [TGs, TSb], [Tpb])
                        a_, Ta_ = aR.next()
                        self.copy("act", a_[:, :], pa[:, :], [Tpa], [Ta_])
                        self.tt("dve", fp[:, cq, :], a_[:, :], pb[:, :], ALU.add, [Ta_, Tpb], [Tfp])
                        self.tt("dve", fm[:, cq, :], a_[:, :], pb[:, :], ALU.subtract, [Ta_, Tpb], [Tfm])
                    if pend is not None:
                        ystage(*pend)
                    pend = (kb, fp, Tfp, fm, Tfm, wf, Twf, yst, Tyst)
                    for _ in range(per_kb):
                        if cops:
                            cops.pop(0)()
                ystage(*pend)
                pend = None
                while cops:
                    cops.pop(0)()
                for cq in range(2):
                    pa, Tpa = paR.next()
                    for i in range(32):
                        self.mm(pa[:, 0:2], Gs[:, i, cq * 128:(cq + 1) * 128], nyq[:, i, :], i == 0, i == 31,
                                [TGs, Tnyq], [Tpa])
                    self.copy("act", fan[:, cq, :], pa[:, 0:2], [Tpa], [Tfan])
                for dq in range(2):
                    py, Tpy = pyR.next()
                    for cq in range(2):
                        self.mm(py[:, 0:2], wf[:, cq, dq * 128:(dq + 1) * 128], fan[:, cq, :], cq == 0, cq == 1,
                                [Twf, Tfan], [Tpy])
                    self.copy("act", yst[:, dq, L // 2:L // 2 + 1], py[:, 0:1], [Tpy], [Tyst])
                self.dma("pool", self.yT[g * 256:(g + 1) * 256, :].rearrange("(q p) t -> p q t", p=128), yst[:, :, :],
                         [Tyst], [])
                yb, Tyb = ybR.next()
                for tb in range(8):
                    for dq in range(2):
                        pp, Tpp = ppR.next()
                        for cc in range(2):
                            self.mm(pp[:, :], wp[:, cc, dq * 128:(dq + 1) * 128], pl[:, cc, tb * 512:(tb + 1) * 512],
                                    cc == 0, cc == 1, [Twp, Tpl], [Tpp])
                        self.ts("dve", yb[:, dq, tb * 512:(tb + 1) * 512], pp[:, :], psc[:, g, dq:dq + 1], None,
                                ALU.mult, None, [Tpp, Tpsc], [Tyb])
                r0 = 1024 + g * 256
                self.dma("pool", self.yT[r0:r0 + 256, :].rearrange("(q p) t -> p q t", p=128), yb[:, :, :], [Tyb], [])

    def phase_D(self, l, xsrc):
        S = self.S
        with ExitStack() as es:
            wout, _ = self.sb(es, "wout", [128, 16, D], BF16)
            Twout = [S.tile("woutq%d" % q) for q in range(4)]
            gb, Tgb = self.sb(es, "gbD", [128, D], F32)
            wr, Twr = self.sb(es, "wrD", [128, 16, NE], F32)
            junk, Tjunk = self.sb(es, "junkD", [128, D], BF16)
            yTR = self.ring(es, "yTD", 2, [128, 16, 512], BF16)
            xtR = self.ring(es, "xtD", 2, [128, D], F32)
            xnR = self.ring(es, "xnD", 2, [128, D], F32)
            hfR = self.ring(es, "hfD", 2, [128, D], F32)
            hbR = self.ring(es, "hbD", 2, [128, D], BF16)
            hTR = self.ring(es, "hTD", 2, [128, 16, 128], F32)
            small = self.ring(es, "smD", 32, [128, 1], F32)
            exR = self.ring(es, "exD", 2, [128, NE], F32)
            poR = self.ring(es, "poD", 3, [128, 512], F32, psum=True)
            ptR = self.ring(es, "ptD", 4, [128, 4, 128], F32, psum=True)
            plR = self.ring(es, "plD", 1, [128, NE], F32, psum=True)

            wsrc = self.w_out[l].rearrange("(c p) f -> p c f", p=128)
            for q in range(4):
                self.dma("pool", wout[:, :, q * 512:(q + 1) * 512], wsrc[:, :, q * 512:(q + 1) * 512], [], [Twout[q]])
            self.dma("sp", gb[:, :], self.norm2_g[l].partition_broadcast(128), [], [Tgb])
            self.dma("sp", wr[:, :, :], self.w_router[l].rearrange("(c p) e -> p c e", p=128), [], [Twr])
            yview = self.yT.rearrange("(c p) t -> p c t", p=128)
            state = {"yTb": None}

            def load_y(tb):
                yTb, TyTb = yTR.next()
                self.dma("sp", yTb[:, :, :], yview[:, :, tb * 512:(tb + 1) * 512], [], [TyTb])
                state[tb] = (yTb, TyTb)

            load_y(0)

            def stage1(ti):
                tb, i = ti // 4, ti % 4
                if i == 0 and tb + 1 < 8:
                    load_y(tb + 1)
                yTb, TyTb = state[tb]
                t0 = ti * 128
                xt, Txt = xtR.next()
                self.dma("sp", xt[:, :], xsrc[t0:t0 + 128, :], [], [Txt])
                xn, Txn = xnR.next()
                for db in range(4):
                    po, Tpo = poR.next()
                    for mc in range(16):
                        self.mm(po[:, :], yTb[:, mc, i * 128:(i + 1) * 128], wout[:, mc, db * 512:(db + 1) * 512],
                                mc == 0, mc == 15, [TyTb, Twout[db]], [Tpo])
                    self.tt("dve", xn[:, db * 512:(db + 1) * 512], po[:, :], xt[:, db * 512:(db + 1) * 512],
                            ALU.add, [Tpo, Txt], [Txn])
                self.dma("pool", self.xres[t0:t0 + 128, :], xn[:, :], [Txn], [])
                rs, Trs = self.rstd_of(xn, Txn, junk, Tjunk, small)
                hf, Thf = hfR.next()
                self.stt(hf[:, :], xn[:, :], rs[:, 0:1], gb[:, :], ALU.mult, ALU.mult, [Txn, Trs, Tgb], [Thf])
                hb, Thb = hbR.next()
                self.copy("act", hb[:, :], hf[:, :], [Thf], [Thb])
                self.dma("pool", self.h2d[t0:t0 + 128, :], hb[:, :], [Thb], [])
                return hf, Thf

            def stage2(hfp):
                hf, Thf = hfp
                hT, ThT = hTR.next()
                for q in range(4):
                    pt, Tpt = ptR.next()
                    for c in range(4):
                        cc = q * 4 + c
                        self.tr(pt[:, c, :], hf[:, cc * 128:(cc + 1) * 128], self.identf[:, :],
                                [Thf, self.Tidentf], [Tpt])
                    self.copy("act" if q % 2 == 0 else "dve", hT[:, q * 4:(q + 1) * 4, :], pt[:, :, :], [Tpt], [ThT])
                return hT, ThT

            def stage3(ti, hTp):
                hT, ThT = hTp
                pl, Tpl = plR.next()
                for dc in range(16):
                    self.mm(pl[:, :], hT[:, dc, :], wr[:, dc, :], dc == 0, dc == 15, [ThT, Twr], [Tpl])
                mx, Tmx = small.next()
                nmx, Tnmx = small.next()
                sm, Tsm = small.next()
                rsm, Trsm = small.next()
                ex, Tex = exR.next()
                self.S.add("dve", lambda e, mx=mx, pl=pl: e.reduce_max(out=mx[:, 0:1], in_=pl[:, :], axis=AX.X),
                           [Tpl], [Tmx])
                self.ts("dve", nmx[:, 0:1], mx[:, 0:1], -1.0, None, ALU.mult, None, [Tmx], [Tnmx])
                self.act(ex[:, :], pl[:, :], AF.Exp, [Tpl, Tnmx], [Tex, Tsm], bias=nmx[:, 0:1], scale=1.0,
                         accum_out=sm[:, 0:1])
                self.S.add("dve", lambda e, rsm=rsm, sm=sm: e.reciprocal(out=rsm[:, 0:1], in_=sm[:, 0:1]),
                           [Tsm], [Trsm])
                self.ts("dve", self.P_all[:, ti, :], ex[:, :], rsm[:, 0:1], None, ALU.mult, None, [Tex, Trsm],
                        [self.TP])

            hf_cur = stage1(0)
            hT_prev = None
            for ti in range(NT):
                hf_next = stage1(ti + 1) if ti + 1 < NT else None
                hT_cur = stage2(hf_cur)
                if hT_prev is not None:
                    stage3(ti - 1, hT_prev)
                hT_prev = hT_cur
                hf_cur = hf_next
            stage3(NT - 1, hT_prev)

    def phase_E(self, l):
        S = self.S
        NIT = 22
        with ExitStack() as es:
            Pp, TPp = self.sb(es, "PpE", [128, 4, NE, 8], F32)
            PT, TPT = self.sb(es, "PT", [128, 512], F32)
            mk, Tmk = self.sb(es, "mkE", [128, 512], F32)
            cum, Tcum = self.sb(es, "cumE", [128, 512], F32)
            ones, Tones = self.sb(es, "onesE", [128, 512], F32)
            jk, Tjk = self.sb(es, "jkE", [128, 512], F32)
            blk, Tblk = self.sb(es, "blkE", [128, 128], F32)
            ltri, Tltri = self.sb(es, "ltriE", [128, 128], F32)
            io16, Tio16 = self.sb(es, "io16E", [128, NE, 128], F32)
            tokf, Ttokf = self.sb(es, "tokfE", [128, NT, NE], F32)
            sdiv, Tsdiv = self.sb(es, "sdivE", [128, NT, NE], F32)
            smod, Tsmod = self.sb(es, "smodE", [128, NT, NE], F32)
            rhs8, Trhs8 = self.sb(es, "rhs8E", [128, NT, NE, 4, 2], F32)
            lo, Tlo = self.sb(es, "loE", [128, 1], F32)
            segt, Tsegt = self.sb(es, "segtE", [128, 2], F32)
            candR = self.ring(es, "candE", 2, [128, 1], F32)
            cntR = self.ring(es, "cntE", 2, [128, 2], F32)
            stpR = self.ring(es, "stpE", 2, [128, 1], F32)
            ohR = self.ring(es, "ohE", 3, [128, NE, 128], F32)
            ptp, Tptp = self.ps(es, "ptpE", [128, 512], F32)
            totR = self.ring(es, "totE", 2, [128, 2], F32, psum=True)
            pof, Tpof = self.ps(es, "pofE", [128, 2], F32)
            psl, Tpsl = self.ps(es, "pslE", [128, 4, NE, 8], F32)
            pig, Tpig = self.ps(es, "pigE", [128, NE, 4, 2], F32)

            self.dma("sp", blk[:, :], self.c_blk[:, :], [], [Tblk])
            self.dma("sp", ltri[:, :], self.c_ltri[:, :], [], [Tltri])
            self.dma("sp", io16[:, :, :], self.c_io16.rearrange("p (e b) -> p e b", b=128), [], [Tio16])
            self.dma("sp", tokf[:, :, :], self.c_tokf.rearrange("p (i e) -> p i e", e=NE), [], [Ttokf])
            self.memset("pool", ones[:, :], 1.0, [Tones])
            self.memset("dve", lo[:, :], 0.0, [Tlo])
            self.memset("dve", segt[:, :], 0.0, [Tsegt])
            for (c_, Tc_) in cntR.items:
                self.memset("dve", c_[:, :], 0.0, [Tc_])
            self.copy("dve", Pp[:, :, :, :], self.P_all[:, :, :].rearrange("p (s j) e -> p j e s", j=4),
                      [self.TP], [TPp])
            for jc in range(4):
                self.tr(ptp[:, jc * 128:(jc + 1) * 128], Pp[:, jc, :, :].rearrange("p e s -> p (e s)"),
                        self.identf[:, :], [TPp, self.Tidentf], [Tptp])
            self.copy("dve", PT[:, :], ptp[:, :], [Tptp], [TPT])
            step = 0.5
            for it in range(NIT):
                cand, Tcand = candR.next()
                cnt, Tcnt = cntR.next()
                stp, Tstp = stpR.next()
                tot, Ttot = totR.next()
                self.ts("dve", cand[:, 0:1], lo[:, 0:1], step, None, ALU.add, None, [Tlo], [Tcand])
                self.ts("dve", jk[:, :], PT[:, :], cand[:, 0:1], 0.0, ALU.is_ge, ALU.add, [TPT, Tcand], [Tjk, Tcnt],
                        accum_out=cnt[:, 0:1])
                self.mm(tot[:, 0:2], blk[:, :], cnt[:, 0:2], True, True, [Tblk, Tcnt], [Ttot])
                self.ts("dve", stp[:, 0:1], tot[:, 0:1], float(CAP) - 0.5, step, ALU.is_ge, ALU.mult, [Ttot], [Tstp])
                self.tt("dve", lo[:, 0:1], lo[:, 0:1], stp[:, 0:1], ALU.add, [Tstp, Tlo], [Tlo])
                step *= 0.5
            self.ts("dve", mk[:, :], PT[:, :], lo[:, 0:1], None, ALU.is_ge, None, [TPT, Tlo], [Tmk])
            S.add("dve", lambda e: e.tensor_tensor_scan(out=cum[:, :], data0=ones[:, :], data1=mk[:, :], initial=0.0,
                                                        op0=ALU.mult, op1=ALU.add), [Tones, Tmk], [Tcum])
            self.copy("dve", segt[:, 0:1], cum[:, 511:512], [Tcum], [Tsegt])
            self.mm(pof[:, 0:2], ltri[:, :], segt[:, 0:2], True, True, [Tltri, Tsegt], [Tpof])
            self.ts("dve", cum[:, :], cum[:, :], pof[:, 0:1], None, ALU.add, None, [Tcum, Tpof], [Tcum])
            self.tt("dve", cum[:, :], cum[:, :], mk[:, :], ALU.mult, [Tcum, Tmk], [Tcum])
            self.ts("dve", cum[:, :], cum[:, :], -1.0, None, ALU.add, None, [Tcum], [Tcum])
            for jc in range(4):
                self.tr(psl[:, jc, :, :].rearrange("p e s -> p (e s)"), cum[:, jc * 128:(jc + 1) * 128],
                        self.identf[:, :], [Tcum, self.Tidentf], [Tpsl])
            self.copy("dve", self.slot_all[:, :, :].rearrange("p (s j) e -> p j e s", j=4), psl[:, :, :, :],
                      [Tpsl], [self.Tslot])
            sl = self.slot_all
            self.ts("dve", sdiv[:, :, :], sl[:, :, :], 128.0, None, ALU.is_ge, None, [self.Tslot], [Tsdiv])
            self.stt(sdiv[:, :, :], sl[:, :, :], 256.0, sdiv[:, :, :], ALU.is_ge, ALU.add, [self.Tslot, Tsdiv], [Tsdiv])
            self.stt(sdiv[:, :, :], sl[:, :, :], 384.0, sdiv[:, :, :], ALU.is_ge, ALU.add, [self.Tslot, Tsdiv], [Tsdiv])
            self.stt(smod[:, :, :], sdiv[:, :, :], -128.0, sl[:, :, :], ALU.mult, ALU.add, [self.Tslot, Tsdiv], [Tsmod])
            for a_ in range(4):
                self.stt(rhs8[:, :, :, a_, 0], sdiv[:, :, :], float(a_), tokf[:, :, :], ALU.is_equal, ALU.mult,
                         [Tsdiv, Ttokf], [Trhs8])
                self.stt(rhs8[:, :, :, a_, 1], sdiv[:, :, :], float(a_), self.P_all[:, :, :], ALU.is_equal, ALU.mult,
                         [Tsdiv, self.TP], [Trhs8])
            for i in range(NT):
                oh, Toh = ohR.next()
                self.tt("dve", oh[:, :, :], io16[:, :, :], smod[:, i, :].unsqueeze(2).to_broadcast([128, NE, 128]),
                        ALU.is_equal, [Tio16, Tsmod], [Toh])
                for e_ in range(NE):
                    self.mm(pig[:, e_, :, :].rearrange("p a c -> p (a c)"), oh[:, e_, :],
                            rhs8[:, i, e_, :, :].rearrange("p a c -> p (a c)"),
                            i == 0 and e_ == 0, i == NT - 1 and e_ == NE - 1, [Toh, Trhs8], [Tpig])
            self.copy("dve", self.idx_all[:, :, :], pig[:, :, :, 0], [Tpig], [self.Tidx])
            self.copy("dve", self.gate_all[:, :, :], pig[:, :, :, 1], [Tpig], [self.Tgate])

    def phase_F(self, l):
        S = self.S
        with ExitStack() as es:
            wR = self.wR
            xs, Txs = self.xsF
            xsT, TxsT = self.sb(es, "xsTF", [128, 16, CAP], BF16)
            hid, Thid = self.sb(es, "hidF", [128, 16, CAP], BF16)
            sgR = self.ring(es, "sgF", 2, [128, CAP], F32)
            ysR = self.ring(es, "ysF", 4, [128, D], F32)
            ptR = self.ring(es, "ptF", 2, [128, 8, 128], BF16, psum=True)
            pgR = self.ring(es, "pgF", 2, [128, CAP], F32, psum=True)
            puR = self.ring(es, "puF", 2, [128, CAP], F32, psum=True)
            pdR = self.ring(es, "pdF", 2, [128, 512], F32, psum=True)
            Txres = S.tile("xres_scatter")

            def wload(src2d, blk):
                wt, Twt = wR.next()
                self.dma("pool", wt[:, :, :], src2d.rearrange("(c p) f -> p c f", p=128)[:, :, blk * 512:(blk + 1) * 512],
                         [], [Twt])
                return wt, Twt

            NPRE = 6

            def units_of(e_):
                units = []
                for fb in range(4):
                    units.append((self.w_gate[l, e_], fb))
                    units.append((self.w_up[l, e_], fb))
                for db in range(4):
                    units.append((self.w_down[l, e_], db))
                return units

            def gather(e_):
                for sc in range(4):
                    S.add("pool", lambda e, sc=sc, e_=e_: e.indirect_dma_start(
                        out=xs[:, sc, :], out_offset=None, in_=self.h2d[:, :],
                        in_offset=bass.IndirectOffsetOnAxis(ap=self.idx_all[:, e_, sc:sc + 1], axis=0)),
                        [self.Tidx], [Txs], dma=True)

            def transposes():
                for sc in range(4):
                    for half in range(2):
                        pt, Tpt = ptR.next()
                        for c in range(8):
                            cc = half * 8 + c
                            self.tr(pt[:, c, :], xs[:, sc, cc * 128:(cc + 1) * 128], self.identb[:, :],
                                    [Txs, self.Tidentb], [Tpt])
                        self.copy("act" if half == 0 else "dve", xsT[:, half * 8:(half + 1) * 8, sc * 128:(sc + 1) * 128],
                                  pt[:, :, :], [Tpt], [TxsT])

            def gate_up(units, loaded):
                nxt = NPRE
                for fb in range(4):
                    wg, Twg = loaded[2 * fb]
                    wu, Twu = loaded[2 * fb + 1]
                    for fcl in range(4):
                        fc = fb * 4 + fcl
                        pg, Tpg = pgR.next()
                        pu, Tpu = puR.next()
                        for dc in range(16):
                            self.mm(pg[:, :], wg[:, dc, fcl * 128:(fcl + 1) * 128], xsT[:, dc, :], dc == 0, dc == 15,
                                    [Twg, TxsT], [Tpg])
                        for dc in range(16):
                            self.mm(pu[:, :], wu[:, dc, fcl * 128:(fcl + 1) * 128], xsT[:, dc, :], dc == 0, dc == 15,
                                    [Twu, TxsT], [Tpu])
                        sg, Tsg = sgR.next()
                        self.act(sg[:, :], pg[:, :], AF.Silu, [Tpg], [Tsg])
                        self.tt("dve", hid[:, fc, :], sg[:, :], pu[:, :], ALU.mult, [Tsg, Tpu], [Thid])
                    for _ in range(2):
                        if nxt < len(units):
                            loaded.append(wload(*units[nxt]))
                            nxt += 1

            def down(e_, loaded, nu, nxt_loaded):
                ys_list = [ysR.next() for _ in range(4)]
                for db in range(4):
                    wd, Twd = loaded[8 + db]
                    for sc in range(4):
                        pd, Tpd = pdR.next()
                        for fc in range(16):
                            self.mm(pd[:, :], hid[:, fc, sc * 128:(sc + 1) * 128], wd[:, fc, :], fc == 0, fc == 15,
                                    [Thid, Twd], [Tpd])
                        ys, Tys = ys_list[sc]
                        if (db * 4 + sc) % 2 == 0:
                            self.ts("dve", ys[:, db * 512:(db + 1) * 512], pd[:, :], self.gate_all[:, e_, sc:sc + 1],
                                    None, ALU.mult, None, [Tpd, self.Tgate], [Tys])
                        else:
                            self.act(ys[:, db * 512:(db + 1) * 512], pd[:, :], AF.Copy, [Tpd, self.Tgate], [Tys],
                                     scale=self.gate_all[:, e_, sc:sc + 1])
                    if nu is not None and len(nxt_loaded) < NPRE:
                        nxt_loaded.append(wload(*nu[len(nxt_loaded)]))

                def scatter():
                    for sc in range(4):
                        ys, Tys = ys_list[sc]
                        S.add("pool", lambda e, sc=sc, ys=ys: e.indirect_dma_start(
                            out=self.xres[:, :],
                            out_offset=bass.IndirectOffsetOnAxis(ap=self.idx_all[:, e_, sc:sc + 1], axis=0),
                            in_=ys[:, :], in_offset=None, compute_op=ALU.add),
                            [Tys, self.Tidx], [Txres], dma=True)
                return scatter

            gather(0)
            transposes()
            gather(1)
            loaded = list(self.pref)
            pending_scatter = None
            for e_ in range(NE):
                units = units_of(e_)
                gate_up(units, loaded)
                nxt_loaded = None
                if pending_scatter is not None:
                    pending_scatter()
                    pending_scatter = None
                nu = None
                if e_ + 1 < NE:
                    nu = units_of(e_ + 1)
                    nxt_loaded = [wload(*nu[ui]) for ui in range(3)]
                    transposes()
                    if e_ + 2 < NE:
                        gather(e_ + 2)
                pending_scatter = down(e_, loaded, nu, nxt_loaded)
                assert nu is None or len(nxt_loaded) == NPRE
                loaded = nxt_loaded
            pending_scatter()

    def phase_G(self):
        with ExitStack() as es:
            gb, Tgb = self.sb(es, "gbG", [128, D], F32)
            junk, Tjunk = self.sb(es, "junkG", [128, D], BF16)
            xtR = self.ring(es, "xtG", 6, [128, D], F32)
            oR = self.ring(es, "oG", 6, [128, D], F32)
            small = self.ring(es, "smG", 24, [128, 1], F32)
            self.dma("sp", gb[:, :], self.final_g[0].partition_broadcast(128), [], [Tgb])
            for ti in range(NT):
                t0 = ti * 128
                xt, Txt = xtR.next()
                self.dma("sp", xt[:, :], self.xres[t0:t0 + 128, :], [], [Txt])
                rs, Trs = self.rstd_of(xt, Txt, junk, Tjunk, small)
                o, To = oR.next()
                self.stt(o[:, :], xt[:, :], rs[:, 0:1], gb[:, :], ALU.mult, ALU.mult, [Txt, Trs, Tgb], [To])
                self.dma("pool", self.out[t0:t0 + 128, :], o[:, :], [To], [])


_CONST = {}


def _constants():
    if _CONST:
        return _CONST
    bf = ml_dtypes.bfloat16
    c = {}
    c["c_identb"] = np.eye(128, dtype=np.float32).astype(bf)
    c["c_identf"] = np.eye(128, dtype=np.float32)
    n = np.arange(256)
    ang = 2.0 * np.pi * ((n[:, None] * n[None, :]) % 256) / 256.0
    sc = 1.0 / 1024.0
    c["c_cs"] = np.concatenate([np.cos(ang) * sc, -np.sin(ang) * sc], axis=1).astype(np.float32).astype(bf)
    t = np.arange(L, dtype=np.int64)
    tk = (t[:, None] * t[None, :]) % L
    ang = (2.0 * np.pi / L) * tk
    cm = np.cos(ang).astype(np.float32)
    sm = np.sin(ang).astype(np.float32)
    def tile_dft(m):
        m4 = np.ascontiguousarray(m[:, :L // 2]).reshape(32, 128, 8, 256)
        return np.ascontiguousarray(m4.transpose(2, 1, 0, 3)).reshape(8, 128, 32 * 256).astype(bf)
    c["c_dftc"] = tile_dft(cm)
    c["c_dfts"] = tile_dft(sm)
    sgn = np.where((np.arange(L) % 2) == 0, 1.0, -1.0).astype(np.float32).reshape(32, 128).T
    c["c_nyq"] = np.ascontiguousarray(np.repeat(sgn[:, :, None], 2, axis=2)).reshape(128, 64).astype(bf)
    inv = np.zeros((4, 16), np.float32)
    for gi, w in enumerate((2, 4, 8, 16)):
        for j in range(8):
            tt = j
            lo = max(tt - w // 2, 0); hi = min(tt + w - w // 2, L)
            inv[gi, j] = 1.0 / (hi - lo)
            tt = L - 8 + j
            lo = max(tt - w // 2, 0); hi = min(tt + w - w // 2, L)
            inv[gi, 8 + j] = 1.0 / (hi - lo)
    c["c_invedge"] = inv.reshape(1, 64)
    q = np.arange(128)
    c["c_blk"] = (q[:, None] // 8 == q[None, :] // 8).astype(np.float32)
    c["c_ltri"] = ((q[:, None] // 8 == q[None, :] // 8) & (q[:, None] % 8 < q[None, :] % 8)).astype(np.float32)
    c["c_io16"] = np.tile(np.arange(128, dtype=np.float32)[None, :], (128, NE))
    tok = (np.arange(NT, dtype=np.float32)[None, :] * 128 + np.arange(128, dtype=np.float32)[:, None])
    c["c_tokf"] = np.ascontiguousarray(np.repeat(tok[:, :, None], NE, axis=2)).reshape(128, NT * NE)
    _CONST.update(c)
    return _CONST


_NC = {}


def _get_nc():
    if "nc" not in _NC:
        _NC["nc"] = Builder().build()
    return _NC["nc"]


def kernel(x, norm1_g, w_in, w_fourier, w_pool, pool_scale, w_out, norm2_g, w_router, w_gate, w_up, w_down,
           final_g):
    f = lambda a: np.ascontiguousarray(np.asarray(a, dtype=np.float32))
    shared = {
        "norm1_g": f(norm1_g), "w_in": f(w_in), "w_fourier": f(w_fourier), "w_pool": f(w_pool),
        "pool_scale": f(pool_scale), "w_out": f(w_out), "norm2_g": f(norm2_g), "w_router": f(w_router),
        "w_gate": f(w_gate), "w_up": f(w_up), "w_down": f(w_down), "final_g": f(final_g).reshape(1, D),
    }
    shared.update(_constants())
    x = f(x)
    nc = _get_nc()
    in_maps = []
    for c in range(NCORES):
        m = dict(shared)
        m["x"] = x[c]
        in_maps.append(m)
    res = run_bass_kernel_spmd(nc, in_maps, core_ids=list(range(NCORES)))
    return np.stack([res.results[c]["out"] for c in range(NCORES)], axis=0).astype(np.float32)
```

```python
import numpy as np
import ml_dtypes
from contextlib import ExitStack
import concourse.bass as bass
import concourse.mybir as mybir
from concourse.bass_utils import run_bass_kernel_spmd

F32 = mybir.dt.float32
BF16 = mybir.dt.bfloat16
U32 = mybir.dt.uint32
ALU = mybir.AluOpType
AF = mybir.ActivationFunctionType
AX = mybir.AxisListType

L = 4096
D = 2048
NL = 2
NE = 16
CAP = 512
NT = L // 128
EPS = 1e-6
NCORES = 4


class Tl:
    __slots__ = ("name", "w", "r", "rd")

    def __init__(self, name):
        self.name = name
        self.w = None
        self.r = {}
        self.rd = []


class Op:
    __slots__ = ("eng", "fn", "dma", "waits", "idx", "needs_inc", "sem", "val", "prev", "incval")


class Sched:
    ENGS = ("pe", "act", "dve", "pool", "sp")

    def __init__(self, nc, csem, pools):
        self.nc = nc
        self.csem = csem
        self.pools = pools
        self.ops = {e: [] for e in self.ENGS}
        self.waited = {e: {} for e in self.ENGS}
        self.dma_waited = {e: set() for e in self.ENGS}
        self.dma_count = {e: 0 for e in self.ENGS}
        self.last_compute = {e: None for e in self.ENGS}
        self.outstanding = []
        self.tiles = []

    def tile(self, name):
        t = Tl(name)
        self.tiles.append(t)
        return t

    def add(self, eng, fn, reads=(), writes=(), dma=False):
        op = Op()
        op.eng = eng
        op.fn = fn
        op.dma = dma
        op.idx = len(self.ops[eng])
        op.needs_inc = False
        op.sem = None
        op.val = 0
        op.prev = None
        op.incval = 0
        deps = []
        for t in reads:
            if t.w is not None:
                deps.append(t.w)
        for t in writes:
            if t.w is not None:
                deps.append(t.w)
            deps.extend(t.r.values())
            deps.extend(t.rd)
        waits = []
        for d in deps:
            if d is op:
                continue
            if d.dma:
                if d in self.dma_waited[eng]:
                    continue
                self.dma_waited[eng].add(d)
                waits.append(d)
            else:
                if eng == "pe" and d.eng == "pe" and not dma:
                    continue
                if self.waited[eng].get(d.eng, -1) >= d.idx:
                    continue
                self.waited[eng][d.eng] = d.idx
                d.needs_inc = True
                waits.append(d)
        if dma:
            pool = self.pools[eng]
            k = self.dma_count[eng]
            op.sem = pool[k % len(pool)]
            uses = k // len(pool)
            op.val = 16 * (uses + 1)
            op.prev = (op.sem, 16 * uses) if uses > 0 else None
            self.dma_count[eng] += 1
            self.outstanding.append(op)
        else:
            self.last_compute[eng] = op
        op.waits = waits
        for t in reads:
            if dma:
                t.rd.append(op)
            else:
                t.r[eng] = op
        for t in writes:
            t.w = op
            t.r = {}
            t.rd = []
        self.ops[eng].append(op)
        return op

    def barrier(self):
        best = {}
        for d in self.outstanding:
            key = id(d.sem)
            if key not in best or best[key].val < d.val:
                best[key] = d
        for eng in self.ENGS:
            waits = []
            for e2 in self.ENGS:
                j = self.last_compute[e2]
                if j is None:
                    continue
                if self.waited[eng].get(e2, -1) >= j.idx:
                    continue
                self.waited[eng][e2] = j.idx
                j.needs_inc = True
                waits.append(j)
            for d in best.values():
                waits.append(d)
            op = Op()
            op.eng = eng
            op.fn = None
            op.dma = False
            op.idx = len(self.ops[eng])
            op.needs_inc = False
            op.sem = None
            op.val = 0
            op.prev = None
            op.incval = 0
            op.waits = waits
            self.ops[eng].append(op)
            self.dma_waited[eng] = set()
        self.outstanding = []
        for t in self.tiles:
            t.w = None
            t.r = {}
            t.rd = []

    def emit(self, block):
        for eng in self.ENGS:
            c = 0
            for op in self.ops[eng]:
                if op.needs_inc:
                    c += 1
                    op.incval = c

        def run(eng, e):
            for op in self.ops[eng]:
                w = {}
                if op.dma and op.prev is not None:
                    w[id(op.prev[0])] = (op.prev[0], op.prev[1])
                for d in op.waits:
                    if d.dma:
                        s, v = d.sem, d.val
                    else:
                        s, v = self.csem[d.eng], d.incval
                    if id(s) not in w or w[id(s)][1] < v:
                        w[id(s)] = (s, v)
                for s, v in w.values():
                    e.wait_ge(s, v)
                if op.fn is not None:
                    ins = op.fn(e)
                    if op.dma:
                        ins.then_inc(op.sem, 16)
                    elif op.needs_inc:
                        ins.then_inc(self.csem[eng], 1)

        block.tensor(lambda e: run("pe", e))
        block.scalar(lambda e: run("act", e))
        block.vector(lambda e: run("dve", e))
        block.gpsimd(lambda e: run("pool", e))
        block.sync(lambda e: run("sp", e))


class Ring:
    def __init__(self, items):
        self.items = items
        self.k = 0

    def next(self):
        it = self.items[self.k % len(self.items)]
        self.k += 1
        return it


class Builder:
    def __init__(self, nlayers=NL, stop_after=None, debug=False, unit=None):
        self.unit = unit
        self.nlayers = nlayers
        self.stop_after = stop_after
        self.debug = debug

    def sb(self, es, name, shape, dt):
        self.uid = getattr(self, "uid", 0) + 1
        name = "%s_%d" % (name, self.uid)
        h = es.enter_context(self.nc.sbuf_tensor(name, shape, dt))
        return h, self.S.tile(name)

    def ps(self, es, name, shape, dt):
        self.uid = getattr(self, "uid", 0) + 1
        name = "%s_%d" % (name, self.uid)
        h = es.enter_context(self.nc.psum_tensor(name, shape, dt))
        return h, self.S.tile(name)

    def ring(self, es, name, n, shape, dt, psum=False):
        items = []
        for i in range(n):
            items.append((self.ps if psum else self.sb)(es, "%s%d" % (name, i), shape, dt))
        return Ring(items)

    def dma(self, eng, out, in_, reads, writes, **kw):
        return self.S.add(eng, lambda e: e.dma_start(out=out, in_=in_, **kw), reads, writes, dma=True)

    def mm(self, out, lhsT, rhs, start, stop, reads, writes):
        return self.S.add("pe", lambda e: e.matmul(out, lhsT, rhs, start=start, stop=stop), reads, writes)

    def tr(self, out, in_, ident, reads, writes):
        return self.S.add("pe", lambda e: e.transpose(out, in_, ident), reads, writes)

    def act(self, out, in_, func, reads, writes, **kw):
        return self.S.add("act", lambda e: e.activation(out=out, in_=in_, func=func, **kw), reads, writes)

    def copy(self, eng, out, in_, reads, writes):
        if eng == "act":
            return self.S.add("act", lambda e: e.copy(out=out, in_=in_), reads, writes)
        return self.S.add(eng, lambda e: e.tensor_copy(out=out, in_=in_), reads, writes)

    def tt(self, eng, out, in0, in1, op, reads, writes):
        return self.S.add(eng, lambda e: e.tensor_tensor(out=out, in0=in0, in1=in1, op=op), reads, writes)

    def ts(self, eng, out, in0, s1, s2, op0, op1, reads, writes, accum_out=None):
        if op1 is None:
            return self.S.add(eng, lambda e: e.tensor_scalar(out=out, in0=in0, scalar1=s1, scalar2=None, op0=op0),
                              reads, writes)
        if accum_out is not None:
            return self.S.add(eng, lambda e: e.tensor_scalar(out=out, in0=in0, scalar1=s1, scalar2=s2, op0=op0,
                                                             op1=op1, accum_out=accum_out), reads, writes)
        return self.S.add(eng, lambda e: e.tensor_scalar(out=out, in0=in0, scalar1=s1, scalar2=s2, op0=op0, op1=op1),
                          reads, writes)

    def stt(self, out, in0, scalar, in1, op0, op1, reads, writes):
        return self.S.add("dve", lambda e: e.scalar_tensor_tensor(out=out, in0=in0, scalar=scalar, in1=in1,
                                                                  op0=op0, op1=op1), reads, writes)

    def memset(self, eng, ap, val, writes):
        return self.S.add(eng, lambda e: e.memset(ap, val), (), writes)

    def rstd_of(self, xt, Txt, junk, Tjunk, small):
        ss, Tss = small.next()
        sd, Tsd = small.next()
        rs, Trs = small.next()
        self.act(junk[:, :], xt[:, :], AF.Square, [Txt], [Tjunk, Tss], accum_out=ss[:, 0:1])
        self.act(sd[:, 0:1], ss[:, 0:1], AF.Sqrt, [Tss, self.Teps], [Tsd], scale=1.0 / D, bias=self.eps[:, 0:1])
        self.S.add("dve", lambda e: e.reciprocal(out=rs[:, 0:1], in_=sd[:, 0:1]), [Tsd], [Trs])
        return rs, Trs

    def build(self):
        nc = bass.Bass("TRN2", target_bir_lowering=False)
        self.nc = nc
        def dt_in(name, shape, dt=F32):
            if self.unit and not name.startswith("c_"):
                return None
            return nc.dram_tensor(name, shape, dt, kind="ExternalInput").ap()
        self.x = dt_in("x", [L, D])
        self.norm1_g = dt_in("norm1_g", [NL, D])
        self.w_in = dt_in("w_in", [NL, D, D])
        self.w_fourier = dt_in("w_fourier", [NL, 4, 256, 256])
        self.w_pool = dt_in("w_pool", [NL, 4, 256, 256])
        self.pool_scale = dt_in("pool_scale", [NL, 4, 256])
        self.w_out = dt_in("w_out", [NL, D, D])
        self.norm2_g = dt_in("norm2_g", [NL, D])
        self.w_router = dt_in("w_router", [NL, D, NE])
        self.w_gate = dt_in("w_gate", [NL, NE, D, D])
        self.w_up = dt_in("w_up", [NL, NE, D, D])
        self.w_down = dt_in("w_down", [NL, NE, D, D])
        self.final_g = dt_in("final_g", [1, D])
        self.c_identb = dt_in("c_identb", [128, 128], BF16)
        self.c_identf = dt_in("c_identf", [128, 128])
        self.c_cs = dt_in("c_cs", [256, 512], BF16)
        self.c_dftc = dt_in("c_dftc", [8, 128, 16 * 256], BF16)
        self.c_dfts = dt_in("c_dfts", [8, 128, 16 * 256], BF16)
        self.c_nyq = dt_in("c_nyq", [128, 16 * 2], BF16)
        self.c_alt = dt_in("c_alt", [1, 256], BF16)
        self.c_rev = dt_in("c_rev", [128, 64], U32)
        self.c_invedge = dt_in("c_invedge", [1, 4 * 16])
        self.c_blk = dt_in("c_blk", [128, 128])
        self.c_ltri = dt_in("c_ltri", [128, 128])
        self.c_io16 = dt_in("c_io16", [128, NE * 128])
        self.c_tokf = dt_in("c_tokf", [128, NT * NE])
        self.out = nc.dram_tensor("out", [L, D] if not self.unit else [128, 8], F32, kind="ExternalOutput").ap()
        if self.unit:
            self.u_p = nc.dram_tensor("u_p", [128, NT * NE], F32, kind="ExternalInput").ap()
        self.xres = nc.dram_tensor("xres", [L, D], F32).ap()
        self.Gd = nc.dram_tensor("Gd", [4, L, 512], BF16).ap()
        self.uBd = nc.dram_tensor("uBd", [1024, L], F32).ap()
        self.yT = nc.dram_tensor("yT", [D, L], BF16).ap()
        self.h2d = nc.dram_tensor("h2d", [L, D], BF16).ap()
        if self.debug:
            self.dbg_yT = nc.dram_tensor("dbg_yT", [D, L], BF16, kind="ExternalOutput").ap()
            self.dbg_x = nc.dram_tensor("dbg_x", [L, D], F32, kind="ExternalOutput").ap()
            self.dbg_p = nc.dram_tensor("dbg_p", [128, NT * NE], F32, kind="ExternalOutput").ap()
            self.dbg_idx = nc.dram_tensor("dbg_idx", [128, NE * 4], U32, kind="ExternalOutput").ap()
            self.dbg_gate = nc.dram_tensor("dbg_gate", [128, NE * 4], F32, kind="ExternalOutput").ap()

        with ExitStack() as top:
            csem = {}
            for e in Sched.ENGS[:4]:
                csem[e] = top.enter_context(nc.semaphore("c_" + e))
            pools = {}
            for e, n in (("sp", 40), ("pool", 24), ("act", 12), ("dve", 12)):
                pools[e] = [top.enter_context(nc.semaphore("d_%s%d" % (e, i))) for i in range(n)]
            self.S = Sched(nc, csem, pools)
            block = top.enter_context(nc.Block())
            with ExitStack() as es:
                self.identb, self.Tidentb = self.sb(es, "identb", [128, 128], BF16)
                self.identf, self.Tidentf = self.sb(es, "identf", [128, 128], F32)
                self.eps, self.Teps = self.sb(es, "eps", [128, 1], F32)
                self.dma("sp", self.identb[:, :], self.c_identb[:, :], [], [self.Tidentb])
                self.dma("sp", self.identf[:, :], self.c_identf[:, :], [], [self.Tidentf])
                self.memset("dve", self.eps[:, :], EPS, [self.Teps])
                if self.unit == "E":
                    self.unit_E(es)
                else:
                    self.body(es)
                self.S.barrier()
            self.S.emit(block)
        return nc

    def unit_E(self, es0):
        with ExitStack() as es:
            self.P_all, self.TP = self.sb(es, "P_all", [128, NT, NE], F32)
            self.slot_all, self.Tslot = self.sb(es, "slot_all", [128, NT, NE], F32)
            self.idx_all, self.Tidx = self.sb(es, "idx_all", [128, NE, 4], U32)
            self.gate_all, self.Tgate = self.sb(es, "gate_all", [128, NE, 4], F32)
            self.dma("sp", self.P_all[:, :, :], self.u_p.rearrange("p (i e) -> p i e", e=NE), [], [self.TP])
            self.phase_E(0)
            self.S.barrier()
            self.dma("sp", self.dbg_idx[:, :], self.idx_all[:, :, :], [self.Tidx], [])
            self.dma("sp", self.dbg_gate[:, :], self.gate_all[:, :, :], [self.Tgate], [])

    def body(self, es0):
        S = self.S
        for l in range(self.nlayers):
            xsrc = self.x if l == 0 else self.xres
            self.phase_A(l, xsrc)
            S.barrier()
            if self.stop_after == ("A", l):
                return
            self.phase_BC(l)
            S.barrier()
            if self.stop_after == ("C", l):
                self.dma("sp", self.dbg_yT[:, :], self.yT[:, :], [], [])
                return
            with ExitStack() as es:
                self.P_all, self.TP = self.sb(es, "P_all", [128, NT, NE], F32)
                self.slot_all, self.Tslot = self.sb(es, "slot_all", [128, NT, NE], F32)
                self.idx_all, self.Tidx = self.sb(es, "idx_all", [128, NE, 4], U32)
                self.gate_all, self.Tgate = self.sb(es, "gate_all", [128, NE, 4], F32)
                self.phase_D(l, xsrc)
                S.barrier()
                if self.stop_after == ("D", l):
                    self.dma("sp", self.dbg_x[:, :], self.xres[:, :], [], [])
                    self.dma("sp", self.dbg_p[:, :], self.P_all[:, :, :], [self.TP], [])
                    return
                esw = es.enter_context(ExitStack())
                self.wR = self.ring(esw, "wF", 7, [128, 16, 512], BF16)
                self.xsF = self.sb(esw, "xsF", [128, 4, D], BF16)
                self.pref = []
                units0 = [(self.w_gate[l, 0], 0), (self.w_up[l, 0], 0), (self.w_gate[l, 0], 1), (self.w_up[l, 0], 1),
                          (self.w_gate[l, 0], 2), (self.w_up[l, 0], 2)]
                for (src2d, blk) in units0:
                    wt, Twt = self.wR.next()
                    self.dma("pool", wt[:, :, :],
                             src2d.rearrange("(c p) f -> p c f", p=128)[:, :, blk * 512:(blk + 1) * 512], [], [Twt])
                    self.pref.append((wt, Twt))
                self.phase_E(l)
                S.barrier()
                if self.stop_after == ("E", l):
                    self.dma("sp", self.dbg_x[:, :], self.xres[:, :], [], [])
                    self.dma("sp", self.dbg_p[:, :], self.P_all[:, :, :], [self.TP], [])
                    self.dma("sp", self.dbg_idx[:, :], self.idx_all[:, :, :], [self.Tidx], [])
                    self.dma("sp", self.dbg_gate[:, :], self.gate_all[:, :, :], [self.Tgate], [])
                    return
                self.phase_F(l)
                S.barrier()
            if self.stop_after == ("F", l):
                self.dma("sp", self.dbg_x[:, :], self.xres[:, :], [], [])
                return
        self.phase_G()

    def phase_A(self, l, xsrc):
        S = self.S
        with ExitStack() as es:
            win, _ = self.sb(es, "win", [128, 16, D], BF16)
            Twin = [S.tile("winq%d" % q) for q in range(4)]
            gb, Tgb = self.sb(es, "gb", [128, D], F32)
            cs, Tcs = self.sb(es, "cs", [128, 2, 512], BF16)
            junk, Tjunk = self.sb(es, "junkA", [128, D], BF16)
            xtR = self.ring(es, "xtA", 3, [128, D], F32)
            hbR = self.ring(es, "hbA", 8, [128, D], BF16)
            hTR = self.ring(es, "hTA", 2, [128, 16, 512], BF16)
            uTfR = self.ring(es, "uTfA", 2, [128, 8, 512], BF16)
            upR = self.ring(es, "upA", 4, [128, 512], F32)
            gtR = self.ring(es, "gtA", 4, [128, 512], BF16)
            small = self.ring(es, "smA", 24, [128, 1], F32)
            ptR = self.ring(es, "ptA", 3, [128, 8, 128], BF16, psum=True)
            puR = self.ring(es, "puA", 3, [128, 512], F32, psum=True)
            pgR = self.ring(es, "pgA", 2, [128, 512], F32, psum=True)

            wsrc = self.w_in[l].rearrange("(c p) f -> p c f", p=128)
            for q in range(4):
                self.dma("pool", win[:, :, q * 512:(q + 1) * 512], wsrc[:, :, q * 512:(q + 1) * 512], [], [Twin[q]])
            self.dma("sp", gb[:, :], self.norm1_g[l].partition_broadcast(128), [], [Tgb])
            self.dma("sp", cs[:, :, :], self.c_cs.rearrange("(c p) f -> p c f", p=128), [], [Tcs])

            def norm(tb):
                hbs = []
                for i in range(4):
                    t0 = tb * 512 + i * 128
                    xt, Txt = xtR.next()
                    self.dma("sp", xt[:, :], xsrc[t0:t0 + 128, :], [], [Txt])
                    rs, Trs = self.rstd_of(xt, Txt, junk, Tjunk, small)
                    hb, Thb = hbR.next()
                    self.stt(hb[:, :], xt[:, :], rs[:, 0:1], gb[:, :], ALU.mult, ALU.mult, [Txt, Trs, Tgb], [Thb])
                    hbs.append((hb, Thb))
                return hbs

            def transp(hbs):
                hT, ThT = hTR.next()
                for i in range(4):
                    hb, Thb = hbs[i]
                    for half in range(2):
                        pt, Tpt = ptR.next()
                        for c in range(8):
                            cc = half * 8 + c
                            self.tr(pt[:, c, :], hb[:, cc * 128:(cc + 1) * 128], self.identb[:, :],
                                    [Thb, self.Tidentb], [Tpt])
                        self.copy("act" if half == 0 else "dve", hT[:, half * 8:(half + 1) * 8, i * 128:(i + 1) * 128],
                                  pt[:, :, :], [Tpt], [ThT])
                return hT, ThT

            self.evA = 0

            def mms(tb, hT, ThT):
                uTf, TuTf = uTfR.next()
                for cchunk in range(16):
                    pu, Tpu = puR.next()
                    for dc in range(16):
                        self.mm(pu[:, :], win[:, dc, cchunk * 128:(cchunk + 1) * 128], hT[:, dc, :],
                                dc == 0, dc == 15, [Twin[cchunk // 4], ThT], [Tpu])
                    if cchunk < 8:
                        self.copy("act" if cchunk % 2 == 0 else "dve", uTf[:, cchunk, :], pu[:, :], [Tpu], [TuTf])
                    else:
                        up, Tup = upR.next()
                        self.copy("act" if cchunk % 2 == 0 else "dve", up[:, :], pu[:, :], [Tpu], [Tup])
                        r0 = (cchunk - 8) * 128
                        self.dma("pool", self.uBd[r0:r0 + 128, tb * 512:(tb + 1) * 512], up[:, :], [Tup], [])
                for i in range(4):
                    for g in range(4):
                        pg, Tpg = pgR.next()
                        for cc in range(2):
                            self.mm(pg[:, :], uTf[:, 2 * g + cc, i * 128:(i + 1) * 128], cs[:, cc, :],
                                    cc == 0, cc == 1, [TuTf, Tcs], [Tpg])
                        gt, Tgt = gtR.next()
                        self.copy("act" if self.evA % 2 == 0 else "dve", gt[:, :], pg[:, :], [Tpg], [Tgt])
                        self.evA += 1
                        t0 = tb * 512 + i * 128
                        self.dma("pool", self.Gd[g, t0:t0 + 128, :], gt[:, :], [Tgt], [])

            hb0 = norm(0)
            hb1 = norm(1)
            cur = transp(hb0)
            nxt_hb = hb1
            for tb in range(8):
                hb2 = norm(tb + 2) if tb + 2 < 8 else None
                nxt = transp(nxt_hb) if tb + 1 < 8 else None
                mms(tb, cur[0], cur[1])
                cur = nxt
                nxt_hb = hb2

    def phase_BC(self, l):
        W = L + 32
        with ExitStack() as es:
            GsR = self.ring(es, "GsB", 2, [128, 16, 512], BF16)
            GrR = self.ring(es, "GrB", 2, [128, 16, 512], BF16)
            g1R = self.ring(es, "g1B", 2, [1, 256], BF16)
            CbR = self.ring(es, "CbB", 2, [128, 16, 256], BF16)
            SbR = self.ring(es, "SbB", 2, [128, 16, 256], BF16)
            nyq, Tnyq = self.sb(es, "nyqB", [128, 16, 2], BF16)
            alt, Talt = self.sb(es, "altB", [1, 256], BF16)
            one2, Tone2 = self.sb(es, "one2B", [1, 2], BF16)
            aR = self.ring(es, "aB", 2, [128, 256], F32)
            fpR = self.ring(es, "fpB", 2, [128, 2, 256], BF16)
            fmR = self.ring(es, "fmB", 2, [128, 2, 256], BF16)
            fan, Tfan = self.sb(es, "fanB", [128, 2, 2], BF16)
            ystR = self.ring(es, "ystB", 1, [128, 2, L], BF16)
            wfR = self.ring(es, "wfB", 2, [128, 2, 256], BF16)
            upR = self.ring(es, "upC", 1, [128, W], F32)
            ra, Tra = self.sb(es, "raC", [128, W], F32)
            rb, Trb = self.sb(es, "rbC", [128, W], F32)
            plR = self.ring(es, "plC", 1, [128, 2, L], BF16)
            ybR = self.ring(es, "ybC", 1, [128, 2, L], BF16)
            wpR = self.ring(es, "wpC", 2, [128, 2, 256], BF16)
            psc, Tpsc = self.sb(es, "pscC", [128, 4, 2], F32)
            ied, Tied = self.sb(es, "iedC", [128, 4, 16], F32)
            e1, Te1 = self.sb(es, "e1C", [128, 16], F32)
            paR = self.ring(es, "paB", 2, [128, 256], F32, psum=True)
            pbR = self.ring(es, "pbB", 2, [128, 256], F32, psum=True)
            pyR = self.ring(es, "pyB", 2, [128, 256], F32, psum=True)
            ppR = self.ring(es, "ppC", 2, [128, 512], F32, psum=True)

            for (u_, Tu_) in upR.items:
                self.memset("pool", u_[:, :], 0.0, [Tu_])
            self.memset("dve", ra[:, :], 0.0, [Tra])
            self.memset("dve", rb[:, :], 0.0, [Trb])
            self.dma("sp", nyq[:, :, :], self.c_nyq.rearrange("p (i k) -> p i k", k=2), [], [Tnyq])
            self.dma("sp", alt[:, :], self.c_alt[:, :], [], [Talt])
            self.memset("dve", one2[:, :], 1.0, [Tone2])
            rev, Trev = self.sb(es, "revB", [128, 64], U32)
            self.dma("sp", rev[:, :], self.c_rev[:, :], [], [Trev])
            self.dma("sp", psc[:, :, :], self.pool_scale[l].rearrange("g (q p) -> p g q", p=128), [], [Tpsc],
                     allow_slow_non_contiguous=True)
            self.dma("sp", ied[:, :, :], self.c_invedge[0].partition_broadcast(128).rearrange("p (g k) -> p g k", k=16),
                     [], [Tied])

            def pool_ops(g, pl, Tpl):
                w = (2, 4, 8, 16)[g]
                ops = []
                lo, hi = 8, W - 8
                for cc in range(2):
                    up, Tup = upR.next()
                    r0 = (g * 2 + cc) * 128
                    ops.append(lambda up=up, Tup=Tup, r0=r0: self.dma("sp", up[:, 16:16 + L], self.uBd[r0:r0 + 128, :],
                                                                      [], [Tup]))
                    ops.append(lambda up=up, Tup=Tup: self.tt("dve", ra[:, lo:hi], up[:, lo - 1:hi - 1], up[:, lo:hi],
                                                              ALU.add, [Tup], [Tra]))
                    cur, Tcur, oth, Toth = ra, Tra, rb, Trb
                    sh = 1
                    for lev in range(g):
                        ops.append(lambda cur=cur, Tcur=Tcur, oth=oth, Toth=Toth, sh=sh: self.tt(
                            "dve", oth[:, lo:hi], cur[:, lo - sh:hi - sh], cur[:, lo + sh:hi + sh], ALU.add,
                            [Tcur], [Toth]))
                        cur, Tcur, oth, Toth = oth, Toth, cur, Tcur
                        sh *= 2
                    ops.append(lambda cur=cur, Tcur=Tcur, up=up, Tup=Tup, cc=cc: self.stt(
                        pl[:, cc, :], cur[:, 16:16 + L], 1.0 / w, up[:, 16:16 + L], ALU.mult, ALU.subtract,
                        [Tcur, Tup], [Tpl]))

                    def edges(cur=cur, Tcur=Tcur, up=up, Tup=Tup, cc=cc):
                        self.tt("dve", e1[:, 0:8], cur[:, 16:24], ied[:, g, 0:8], ALU.mult, [Tcur, Tied], [Te1])
                        self.tt("dve", e1[:, 8:16], cur[:, 16 + L - 8:16 + L], ied[:, g, 8:16], ALU.mult,
                                [Tcur, Tied], [Te1])
                        self.tt("dve", pl[:, cc, 0:8], e1[:, 0:8], up[:, 16:24], ALU.subtract, [Te1, Tup], [Tpl])
                        self.tt("dve", pl[:, cc, L - 8:L], e1[:, 8:16], up[:, 16 + L - 8:16 + L], ALU.subtract,
                                [Te1, Tup], [Tpl])
                    ops.append(edges)
                return ops

            def ystage(kb, fp, Tfp, fm, Tfm, wf, Twf, yst, Tyst):
                j0 = 1 if kb == 0 else 0
                mstart = L - kb * 256 - j0
                n = 256 - j0
                for dq in range(2):
                    py, Tpy = pyR.next()
                    for cq in range(2):
                        self.mm(py[:, :], wf[:, cq, dq * 128:(dq + 1) * 128], fp[:, cq, :], cq == 0, cq == 1,
                                [Twf, Tfp], [Tpy])
                    self.copy("act", yst[:, dq, kb * 256:(kb + 1) * 256], py[:, :], [Tpy], [Tyst])
                    py, Tpy = pyR.next()
                    for cq in range(2):
                        self.mm(py[:, :], wf[:, cq, dq * 128:(dq + 1) * 128], fm[:, cq, :], cq == 0, cq == 1,
                                [Twf, Tfm], [Tpy])
                    self.copy("act", yst[:, dq, mstart:mstart - n:-1], py[:, j0:256], [Tpy], [Tyst])

            pend = None
            for g in range(4):
                Gs, TGs = GsR.next()
                Gr, TGr = GrR.next()
                g1, Tg1 = g1R.next()
                self.dma("sp", Gs[:, :, :], self.Gd[g, 0:L // 2, :].rearrange("(i p) f -> p i f", p=128), [], [TGs])
                for i in range(16):
                    self.S.add("pool", lambda e, i=i, g=g, Gr=Gr: e.indirect_dma_start(
                        out=Gr[:, i, :], out_offset=None, in_=self.Gd.rearrange("g t f -> (g t) f"),
                        in_offset=bass.IndirectOffsetOnAxis(ap=rev[:, g * 16 + i:g * 16 + i + 1], axis=0)),
                        [Trev], [TGr], dma=True)
                self.memset("dve", Gr[0:1, 0, :], 0.0, [TGr])
                self.dma("sp", g1[:, :], self.Gd[g, L // 2:L // 2 + 1, 0:256], [], [Tg1])
                self.tt("dve", Gs[:, :, 0:256], Gs[:, :, 0:256], Gr[:, :, 0:256], ALU.add, [TGs, TGr], [TGs])
                self.tt("dve", Gs[:, :, 256:512], Gs[:, :, 256:512], Gr[:, :, 256:512], ALU.subtract, [TGs, TGr], [TGs])
                wf, Twf = wfR.next()
                self.dma("pool", wf[:, :, :], self.w_fourier[l, g].rearrange("(c p) d -> p c d", p=128), [], [Twf])
                wp, Twp = wpR.next()
                self.dma("pool", wp[:, :, :], self.w_pool[l, g].rearrange("(c p) d -> p c d", p=128), [], [Twp])
                yst, Tyst = ystR.next()
                pl, Tpl = plR.next()
                cops = pool_ops(g, pl, Tpl)
                per_kb = (len(cops) + 7) // 8
                for kb in range(8):
                    Cb, TCb = CbR.next()
                    Sb, TSb = SbR.next()
                    self.dma("sp", Cb[:, :, :], self.c_dftc[kb].rearrange("p (i k) -> p i k", k=256), [], [TCb])
                    self.dma("sp", Sb[:, :, :], self.c_dfts[kb].rearrange("p (i k) -> p i k", k=256), [], [TSb])
                    fp, Tfp = fpR.next()
                    fm, Tfm = fmR.next()
                    for cq in range(2):
                        pa, Tpa = paR.next()
                        pb, Tpb = pbR.next()
                        for i in range(16):
                            self.mm(pa[:, :], Gs[:, i, cq * 128:(cq + 1) * 128], Cb[:, i, :], i == 0, False,
                                    [TGs, TCb], [Tpa])
                        self.mm(pa[:, :], g1[0:1, cq * 128:(cq + 1) * 128], alt[0:1, :], False, True,
                                [Tg1, Talt], [Tpa])
                        for i in range(16):
                            self.mm(pb[:, :], Gs[:, i, 256 + cq * 128:256 + (cq + 1) * 128], Sb[:, i, :], i == 0,
                                    i == 15, [TGs, TSb], [Tpb])
                        a_, Ta_ = aR.next()
                        self.copy("act", a_[:, :], pa[:, :], [Tpa], [Ta_])
                        self.tt("dve", fp[:, cq, :], a_[:, :], pb[:, :], ALU.add, [Ta_, Tpb], [Tfp])
                        self.tt("dve", fm[:, cq, :], a_[:, :], pb[:, :], ALU.subtract, [Ta_, Tpb], [Tfm])
                    if pend is not None:
                        ystage(*pend)
                    pend = (kb, fp, Tfp, fm, Tfm, wf, Twf, yst, Tyst)
                    for _ in range(per_kb):
                        if cops:
                            cops.pop(0)()
                ystage(*pend)
                pend = None
                while cops:
                    cops.pop(0)()
                for cq in range(2):
                    pa, Tpa = paR.next()
                    for i in range(16):
                        self.mm(pa[:, 0:2], Gs[:, i, cq * 128:(cq + 1) * 128], nyq[:, i, :], i == 0, False,
                                [TGs, Tnyq], [Tpa])
                    self.mm(pa[:, 0:2], g1[0:1, cq * 128:(cq + 1) * 128], one2[0:1, :], False, True,
                            [Tg1, Tone2], [Tpa])
                    self.copy("act", fan[:, cq, :], pa[:, 0:2], [Tpa], [Tfan])
                for dq in range(2):
                    py, Tpy = pyR.next()
                    for cq in range(2):
                        self.mm(py[:, 0:2], wf[:, cq, dq * 128:(dq + 1) * 128], fan[:, cq, :], cq == 0, cq == 1,
                                [Twf, Tfan], [Tpy])
                    self.copy("act", yst[:, dq, L // 2:L // 2 + 1], py[:, 0:1], [Tpy], [Tyst])
                self.dma("pool", self.yT[g * 256:(g + 1) * 256, :].rearrange("(q p) t -> p q t", p=128), yst[:, :, :],
                         [Tyst], [])
                yb, Tyb = ybR.next()
                for tb in range(8):
                    for dq in range(2):
                        pp, Tpp = ppR.next()
                        for cc in range(2):
                            self.mm(pp[:, :], wp[:, cc, dq * 128:(dq + 1) * 128], pl[:, cc, tb * 512:(tb + 1) * 512],
                                    cc == 0, cc == 1, [Twp, Tpl], [Tpp])
                        self.ts("dve", yb[:, dq, tb * 512:(tb + 1) * 512], pp[:, :], psc[:, g, dq:dq + 1], None,
                                ALU.mult, None, [Tpp, Tpsc], [Tyb])
                r0 = 1024 + g * 256
                self.dma("pool", self.yT[r0:r0 + 256, :].rearrange("(q p) t -> p q t", p=128), yb[:, :, :], [Tyb], [])

    def phase_D(self, l, xsrc):
        S = self.S
        with ExitStack() as es:
            wout, _ = self.sb(es, "wout", [128, 16, D], BF16)
            Twout = [S.tile("woutq%d" % q) for q in range(4)]
            gb, Tgb = self.sb(es, "gbD", [128, D], F32)
            wr, Twr = self.sb(es, "wrD", [128, 16, NE], F32)
            junk, Tjunk = self.sb(es, "junkD", [128, D], BF16)
            yTR = self.ring(es, "yTD", 2, [128, 16, 512], BF16)
            xtR = self.ring(es, "xtD", 2, [128, D], F32)
            xnR = self.ring(es, "xnD", 2, [128, D], F32)
            hfR = self.ring(es, "hfD", 2, [128, D], F32)
            hbR = self.ring(es, "hbD", 2, [128, D], BF16)
            hTR = self.ring(es, "hTD", 2, [128, 16, 128], F32)
            small = self.ring(es, "smD", 32, [128, 1], F32)
            exR = self.ring(es, "exD", 2, [128, NE], F32)
            poR = self.ring(es, "poD", 3, [128, 512], F32, psum=True)
            ptR = self.ring(es, "ptD", 4, [128, 4, 128], F32, psum=True)
            plR = self.ring(es, "plD", 1, [128, NE], F32, psum=True)

            wsrc = self.w_out[l].rearrange("(c p) f -> p c f", p=128)
            for q in range(4):
                self.dma("pool", wout[:, :, q * 512:(q + 1) * 512], wsrc[:, :, q * 512:(q + 1) * 512], [], [Twout[q]])
            self.dma("sp", gb[:, :], self.norm2_g[l].partition_broadcast(128), [], [Tgb])
            self.dma("sp", wr[:, :, :], self.w_router[l].rearrange("(c p) e -> p c e", p=128), [], [Twr])
            yview = self.yT.rearrange("(c p) t -> p c t", p=128)
            state = {"yTb": None}

            def load_y(tb):
                yTb, TyTb = yTR.next()
                self.dma("sp", yTb[:, :, :], yview[:, :, tb * 512:(tb + 1) * 512], [], [TyTb])
                state[tb] = (yTb, TyTb)

            load_y(0)

            def stage1(ti):
                tb, i = ti // 4, ti % 4
                if i == 0 and tb + 1 < 8:
                    load_y(tb + 1)
                yTb, TyTb = state[tb]
                t0 = ti * 128
                xt, Txt = xtR.next()
                self.dma("sp", xt[:, :], xsrc[t0:t0 + 128, :], [], [Txt])
                xn, Txn = xnR.next()
                for db in range(4):
                    po, Tpo = poR.next()
                    for mc in range(16):
                        self.mm(po[:, :], yTb[:, mc, i * 128:(i + 1) * 128], wout[:, mc, db * 512:(db + 1) * 512],
                                mc == 0, mc == 15, [TyTb, Twout[db]], [Tpo])
                    self.tt("dve", xn[:, db * 512:(db + 1) * 512], po[:, :], xt[:, db * 512:(db + 1) * 512],
                            ALU.add, [Tpo, Txt], [Txn])
                self.dma("pool", self.xres[t0:t0 + 128, :], xn[:, :], [Txn], [])
                rs, Trs = self.rstd_of(xn, Txn, junk, Tjunk, small)
                hf, Thf = hfR.next()
                self.stt(hf[:, :], xn[:, :], rs[:, 0:1], gb[:, :], ALU.mult, ALU.mult, [Txn, Trs, Tgb], [Thf])
                hb, Thb = hbR.next()
                self.copy("act", hb[:, :], hf[:, :], [Thf], [Thb])
                self.dma("pool", self.h2d[t0:t0 + 128, :], hb[:, :], [Thb], [])
                return hf, Thf

            def stage2(hfp):
                hf, Thf = hfp
                hT, ThT = hTR.next()
                for q in range(4):
                    pt, Tpt = ptR.next()
                    for c in range(4):
                        cc = q * 4 + c
                        self.tr(pt[:, c, :], hf[:, cc * 128:(cc + 1) * 128], self.identf[:, :],
                                [Thf, self.Tidentf], [Tpt])
                    self.copy("act" if q % 2 == 0 else "dve", hT[:, q * 4:(q + 1) * 4, :], pt[:, :, :], [Tpt], [ThT])
                return hT, ThT

            def stage3(ti, hTp):
                hT, ThT = hTp
                pl, Tpl = plR.next()
                for dc in range(16):
                    self.mm(pl[:, :], hT[:, dc, :], wr[:, dc, :], dc == 0, dc == 15, [ThT, Twr], [Tpl])
                mx, Tmx = small.next()
                nmx, Tnmx = small.next()
                sm, Tsm = small.next()
                rsm, Trsm = small.next()
                ex, Tex = exR.next()
                self.S.add("dve", lambda e, mx=mx, pl=pl: e.reduce_max(out=mx[:, 0:1], in_=pl[:, :], axis=AX.X),
                           [Tpl], [Tmx])
                self.ts("dve", nmx[:, 0:1], mx[:, 0:1], -1.0, None, ALU.mult, None, [Tmx], [Tnmx])
                self.act(ex[:, :], pl[:, :], AF.Exp, [Tpl, Tnmx], [Tex, Tsm], bias=nmx[:, 0:1], scale=1.0,
                         accum_out=sm[:, 0:1])
                self.S.add("dve", lambda e, rsm=rsm, sm=sm: e.reciprocal(out=rsm[:, 0:1], in_=sm[:, 0:1]),
                           [Tsm], [Trsm])
                self.ts("dve", self.P_all[:, ti, :], ex[:, :], rsm[:, 0:1], None, ALU.mult, None, [Tex, Trsm],
                        [self.TP])

            hf_cur = stage1(0)
            hT_prev = None
            for ti in range(NT):
                hf_next = stage1(ti + 1) if ti + 1 < NT else None
                hT_cur = stage2(hf_cur)
                if hT_prev is not None:
                    stage3(ti - 1, hT_prev)
                hT_prev = hT_cur
                hf_cur = hf_next
            stage3(NT - 1, hT_prev)

    def phase_E(self, l):
        S = self.S
        NIT = 22
        with ExitStack() as es:
            Pp, TPp = self.sb(es, "PpE", [128, 4, NE, 8], F32)
            PT, TPT = self.sb(es, "PT", [128, 512], F32)
            mk, Tmk = self.sb(es, "mkE", [128, 512], F32)
            cum, Tcum = self.sb(es, "cumE", [128, 512], F32)
            ones, Tones = self.sb(es, "onesE", [128, 512], F32)
            jk, Tjk = self.sb(es, "jkE", [128, 512], F32)
            blk, Tblk = self.sb(es, "blkE", [128, 128], F32)
            ltri, Tltri = self.sb(es, "ltriE", [128, 128], F32)
            io16, Tio16 = self.sb(es, "io16E", [128, NE, 128], F32)
            tokf, Ttokf = self.sb(es, "tokfE", [128, NT, NE], F32)
            sdiv, Tsdiv = self.sb(es, "sdivE", [128, NT, NE], F32)
            smod, Tsmod = self.sb(es, "smodE", [128, NT, NE], F32)
            rhs8, Trhs8 = self.sb(es, "rhs8E", [128, NT, NE, 4, 2], F32)
            lo, Tlo = self.sb(es, "loE", [128, 1], F32)
            segt, Tsegt = self.sb(es, "segtE", [128, 2], F32)
            candR = self.ring(es, "candE", 2, [128, 1], F32)
            cntR = self.ring(es, "cntE", 2, [128, 2], F32)
            stpR = self.ring(es, "stpE", 2, [128, 1], F32)
            ohR = self.ring(es, "ohE", 3, [128, NE, 128], F32)
            ptp, Tptp = self.ps(es, "ptpE", [128, 512], F32)
            totR = self.ring(es, "totE", 2, [128, 2], F32, psum=True)
            pof, Tpof = self.ps(es, "pofE", [128, 2], F32)
            psl, Tpsl = self.ps(es, "pslE", [128, 4, NE, 8], F32)
            pig, Tpig = self.ps(es, "pigE", [128, NE, 4, 2], F32)

            self.dma("sp", blk[:, :], self.c_blk[:, :], [], [Tblk])
            self.dma("sp", ltri[:, :], self.c_ltri[:, :], [], [Tltri])
            self.dma("sp", io16[:, :, :], self.c_io16.rearrange("p (e b) -> p e b", b=128), [], [Tio16])
            self.dma("sp", tokf[:, :, :], self.c_tokf.rearrange("p (i e) -> p i e", e=NE), [], [Ttokf])
            self.memset("pool", ones[:, :], 1.0, [Tones])
            self.memset("dve", lo[:, :], 0.0, [Tlo])
            self.memset("dve", segt[:, :], 0.0, [Tsegt])
            for (c_, Tc_) in cntR.items:
                self.memset("dve", c_[:, :], 0.0, [Tc_])
            self.copy("dve", Pp[:, :, :, :], self.P_all[:, :, :].rearrange("p (s j) e -> p j e s", j=4),
                      [self.TP], [TPp])
            for jc in range(4):
                self.tr(ptp[:, jc * 128:(jc + 1) * 128], Pp[:, jc, :, :].rearrange("p e s -> p (e s)"),
                        self.identf[:, :], [TPp, self.Tidentf], [Tptp])
            self.copy("dve", PT[:, :], ptp[:, :], [Tptp], [TPT])
            step = 0.5
            for it in range(NIT):
                cand, Tcand = candR.next()
                cnt, Tcnt = cntR.next()
                stp, Tstp = stpR.next()
                tot, Ttot = totR.next()
                self.ts("dve", cand[:, 0:1], lo[:, 0:1], step, None, ALU.add, None, [Tlo], [Tcand])
                self.ts("dve", jk[:, :], PT[:, :], cand[:, 0:1], 0.0, ALU.is_ge, ALU.add, [TPT, Tcand], [Tjk, Tcnt],
                        accum_out=cnt[:, 0:1])
                self.mm(tot[:, 0:2], blk[:, :], cnt[:, 0:2], True, True, [Tblk, Tcnt], [Ttot])
                self.ts("dve", stp[:, 0:1], tot[:, 0:1], float(CAP) - 0.5, step, ALU.is_ge, ALU.mult, [Ttot], [Tstp])
                self.tt("dve", lo[:, 0:1], lo[:, 0:1], stp[:, 0:1], ALU.add, [Tstp, Tlo], [Tlo])
                step *= 0.5
            self.ts("dve", mk[:, :], PT[:, :], lo[:, 0:1], None, ALU.is_ge, None, [TPT, Tlo], [Tmk])
            S.add("dve", lambda e: e.tensor_tensor_scan(out=cum[:, :], data0=ones[:, :], data1=mk[:, :], initial=0.0,
                                                        op0=ALU.mult, op1=ALU.add), [Tones, Tmk], [Tcum])
            self.copy("dve", segt[:, 0:1], cum[:, 511:512], [Tcum], [Tsegt])
            self.mm(pof[:, 0:2], ltri[:, :], segt[:, 0:2], True, True, [Tltri, Tsegt], [Tpof])
            self.ts("dve", cum[:, :], cum[:, :], pof[:, 0:1], None, ALU.add, None, [Tcum, Tpof], [Tcum])
            self.tt("dve", cum[:, :], cum[:, :], mk[:, :], ALU.mult, [Tcum, Tmk], [Tcum])
            self.ts("dve", cum[:, :], cum[:, :], -1.0, None, ALU.add, None, [Tcum], [Tcum])
            for jc in range(4):
                self.tr(psl[:, jc, :, :].rearrange("p e s -> p (e s)"), cum[:, jc * 128:(jc + 1) * 128],
                        self.identf[:, :], [Tcum, self.Tidentf], [Tpsl])
            self.copy("dve", self.slot_all[:, :, :].rearrange("p (s j) e -> p j e s", j=4), psl[:, :, :, :],
                      [Tpsl], [self.Tslot])
            sl = self.slot_all
            self.ts("dve", sdiv[:, :, :], sl[:, :, :], 128.0, None, ALU.is_ge, None, [self.Tslot], [Tsdiv])
            self.stt(sdiv[:, :, :], sl[:, :, :], 256.0, sdiv[:, :, :], ALU.is_ge, ALU.add, [self.Tslot, Tsdiv], [Tsdiv])
            self.stt(sdiv[:, :, :], sl[:, :, :], 384.0, sdiv[:, :, :], ALU.is_ge, ALU.add, [self.Tslot, Tsdiv], [Tsdiv])
            self.stt(smod[:, :, :], sdiv[:, :, :], -128.0, sl[:, :, :], ALU.mult, ALU.add, [self.Tslot, Tsdiv], [Tsmod])
            for a_ in range(4):
                self.stt(rhs8[:, :, :, a_, 0], sdiv[:, :, :], float(a_), tokf[:, :, :], ALU.is_equal, ALU.mult,
                         [Tsdiv, Ttokf], [Trhs8])
                self.stt(rhs8[:, :, :, a_, 1], sdiv[:, :, :], float(a_), self.P_all[:, :, :], ALU.is_equal, ALU.mult,
                         [Tsdiv, self.TP], [Trhs8])
            for i in range(NT):
                oh, Toh = ohR.next()
                self.tt("dve", oh[:, :, :], io16[:, :, :], smod[:, i, :].unsqueeze(2).to_broadcast([128, NE, 128]),
                        ALU.is_equal, [Tio16, Tsmod], [Toh])
                for e_ in range(NE):
                    self.mm(pig[:, e_, :, :].rearrange("p a c -> p (a c)"), oh[:, e_, :],
                            rhs8[:, i, e_, :, :].rearrange("p a c -> p (a c)"),
                            i == 0 and e_ == 0, i == NT - 1 and e_ == NE - 1, [Toh, Trhs8], [Tpig])
            self.copy("dve", self.idx_all[:, :, :], pig[:, :, :, 0], [Tpig], [self.Tidx])
            self.copy("dve", self.gate_all[:, :, :], pig[:, :, :, 1], [Tpig], [self.Tgate])

    def phase_F(self, l):
        S = self.S
        with ExitStack() as es:
            wR = self.wR
            xs, Txs = self.xsF
            xsT, TxsT = self.sb(es, "xsTF", [128, 16, CAP], BF16)
            hid, Thid = self.sb(es, "hidF", [128, 16, CAP], BF16)
            sgR = self.ring(es, "sgF", 2, [128, CAP], F32)
            ysR = self.ring(es, "ysF", 4, [128, D], F32)
            ptR = self.ring(es, "ptF", 2, [128, 8, 128], BF16, psum=True)
            pgR = self.ring(es, "pgF", 2, [128, CAP], F32, psum=True)
            puR = self.ring(es, "puF", 2, [128, CAP], F32, psum=True)
            pdR = self.ring(es, "pdF", 2, [128, 512], F32, psum=True)
            Txres = S.tile("xres_scatter")

            def wload(src2d, blk):
                wt, Twt = wR.next()
                self.dma("pool", wt[:, :, :], src2d.rearrange("(c p) f -> p c f", p=128)[:, :, blk * 512:(blk + 1) * 512],
                         [], [Twt])
                return wt, Twt

            NPRE = 6

            def units_of(e_):
                units = []
                for fb in range(4):
                    units.append((self.w_gate[l, e_], fb))
                    units.append((self.w_up[l, e_], fb))
                for db in range(4):
                    units.append((self.w_down[l, e_], db))
                return units

            def gather(e_):
                for sc in range(4):
                    S.add("pool", lambda e, sc=sc, e_=e_: e.indirect_dma_start(
                        out=xs[:, sc, :], out_offset=None, in_=self.h2d[:, :],
                        in_offset=bass.IndirectOffsetOnAxis(ap=self.idx_all[:, e_, sc:sc + 1], axis=0)),
                        [self.Tidx], [Txs], dma=True)

            def transposes():
                for sc in range(4):
                    for half in range(2):
                        pt, Tpt = ptR.next()
                        for c in range(8):
                            cc = half * 8 + c
                            self.tr(pt[:, c, :], xs[:, sc, cc * 128:(cc + 1) * 128], self.identb[:, :],
                                    [Txs, self.Tidentb], [Tpt])
                        self.copy("act" if half == 0 else "dve", xsT[:, half * 8:(half + 1) * 8, sc * 128:(sc + 1) * 128],
                                  pt[:, :, :], [Tpt], [TxsT])

            def gate_up(units, loaded):
                nxt = NPRE
                for fb in range(4):
                    wg, Twg = loaded[2 * fb]
                    wu, Twu = loaded[2 * fb + 1]
                    for fcl in range(4):
                        fc = fb * 4 + fcl
                        pg, Tpg = pgR.next()
                        pu, Tpu = puR.next()
                        for dc in range(16):
                            self.mm(pg[:, :], wg[:, dc, fcl * 128:(fcl + 1) * 128], xsT[:, dc, :], dc == 0, dc == 15,
                                    [Twg, TxsT], [Tpg])
                        for dc in range(16):
                            self.mm(pu[:, :], wu[:, dc, fcl * 128:(fcl + 1) * 128], xsT[:, dc, :], dc == 0, dc == 15,
                                    [Twu, TxsT], [Tpu])
                        sg, Tsg = sgR.next()
                        self.act(sg[:, :], pg[:, :], AF.Silu, [Tpg], [Tsg])
                        self.tt("dve", hid[:, fc, :], sg[:, :], pu[:, :], ALU.mult, [Tsg, Tpu], [Thid])
                    for _ in range(2):
                        if nxt < len(units):
                            loaded.append(wload(*units[nxt]))
                            nxt += 1

            def down(e_, loaded, nu, nxt_loaded):
                ys_list = [ysR.next() for _ in range(4)]
                for db in range(4):
                    wd, Twd = loaded[8 + db]
                    for sc in range(4):
                        pd, Tpd = pdR.next()
                        for fc in range(16):
                            self.mm(pd[:, :], hid[:, fc, sc * 128:(sc + 1) * 128], wd[:, fc, :], fc == 0, fc == 15,
                                    [Thid, Twd], [Tpd])
                        ys, Tys = ys_list[sc]
                        if (db * 4 + sc) % 2 == 0:
                            self.ts("dve", ys[:, db * 512:(db + 1) * 512], pd[:, :], self.gate_all[:, e_, sc:sc + 1],
                                    None, ALU.mult, None, [Tpd, self.Tgate], [Tys])
                        else:
                            self.act(ys[:, db * 512:(db + 1) * 512], pd[:, :], AF.Copy, [Tpd, self.Tgate], [Tys],
                                     scale=self.gate_all[:, e_, sc:sc + 1])
                    if nu is not None and len(nxt_loaded) < NPRE:
                        nxt_loaded.append(wload(*nu[len(nxt_loaded)]))

                def scatter():
                    for sc in range(4):
                        ys, Tys = ys_list[sc]
                        S.add("pool", lambda e, sc=sc, ys=ys: e.indirect_dma_start(
                            out=self.xres[:, :],
                            out_offset=bass.IndirectOffsetOnAxis(ap=self.idx_all[:, e_, sc:sc + 1], axis=0),
                            in_=ys[:, :], in_offset=None, compute_op=ALU.add),
                            [Tys, self.Tidx], [Txres], dma=True)
                return scatter

            gather(0)
            transposes()
            gather(1)
            loaded = list(self.pref)
            pending_scatter = None
            for e_ in range(NE):
                units = units_of(e_)
                gate_up(units, loaded)
                nxt_loaded = None
                if pending_scatter is not None:
                    pending_scatter()
                    pending_scatter = None
                nu = None
                if e_ + 1 < NE:
                    nu = units_of(e_ + 1)
                    nxt_loaded = [wload(*nu[ui]) for ui in range(3)]
                    transposes()
                    if e_ + 2 < NE:
                        gather(e_ + 2)
                pending_scatter = down(e_, loaded, nu, nxt_loaded)
                assert nu is None or len(nxt_loaded) == NPRE
                loaded = nxt_loaded
            pending_scatter()

    def phase_G(self):
        with ExitStack() as es:
            gb, Tgb = self.sb(es, "gbG", [128, D], F32)
            junk, Tjunk = self.sb(es, "junkG", [128, D], BF16)
            xtR = self.ring(es, "xtG", 6, [128, D], F32)
            oR = self.ring(es, "oG", 6, [128, D], F32)
            small = self.ring(es, "smG", 24, [128, 1], F32)
            self.dma("sp", gb[:, :], self.final_g[0].partition_broadcast(128), [], [Tgb])
            for ti in range(NT):
                t0 = ti * 128
                xt, Txt = xtR.next()
                self.dma("sp", xt[:, :], self.xres[t0:t0 + 128, :], [], [Txt])
                rs, Trs = self.rstd_of(xt, Txt, junk, Tjunk, small)
                o, To = oR.next()
                self.stt(o[:, :], xt[:, :], rs[:, 0:1], gb[:, :], ALU.mult, ALU.mult, [Txt, Trs, Tgb], [To])
                self.dma("pool", self.out[t0:t0 + 128, :], o[:, :], [To], [])


_CONST = {}


def _constants():
    if _CONST:
        return _CONST
    bf = ml_dtypes.bfloat16
    c = {}
    c["c_identb"] = np.eye(128, dtype=np.float32).astype(bf)
    c["c_identf"] = np.eye(128, dtype=np.float32)
    n = np.arange(256)
    ang = 2.0 * np.pi * ((n[:, None] * n[None, :]) % 256) / 256.0
    sc = 1.0 / 1024.0
    c["c_cs"] = np.concatenate([np.cos(ang) * sc, -np.sin(ang) * sc], axis=1).astype(np.float32).astype(bf)
    t = np.arange(L, dtype=np.int64)
    tk = (t[:, None] * t[None, :]) % L
    ang = (2.0 * np.pi / L) * tk
    cm = np.cos(ang).astype(np.float32)
    sm = np.sin(ang).astype(np.float32)
    def tile_dft(m):
        m4 = np.ascontiguousarray(m[:L // 2, :L // 2]).reshape(16, 128, 8, 256)
        return np.ascontiguousarray(m4.transpose(2, 1, 0, 3)).reshape(8, 128, 16 * 256).astype(bf)
    c["c_dftc"] = tile_dft(cm)
    c["c_dfts"] = tile_dft(sm)
    sgn = np.where((np.arange(L // 2) % 2) == 0, 1.0, -1.0).astype(np.float32).reshape(16, 128).T
    c["c_nyq"] = np.ascontiguousarray(np.repeat(sgn[:, :, None], 2, axis=2)).reshape(128, 32).astype(bf)
    rv = np.zeros((128, 4, 16), np.uint32)
    for g_ in range(4):
        for i_ in range(16):
            t_ = i_ * 128 + np.arange(128)
            r_ = (L - t_) % L
            rv[:, g_, i_] = g_ * L + r_
    c["c_rev"] = rv.reshape(128, 64)
    c["c_alt"] = np.where((np.arange(256) % 2) == 0, 1.0, -1.0).astype(np.float32).reshape(1, 256).astype(bf)
    inv = np.zeros((4, 16), np.float32)
    for gi, w in enumerate((2, 4, 8, 16)):
        for j in range(8):
            tt = j
            lo = max(tt - w // 2, 0); hi = min(tt + w - w // 2, L)
            inv[gi, j] = 1.0 / (hi - lo)
            tt = L - 8 + j
            lo = max(tt - w // 2, 0); hi = min(tt + w - w // 2, L)
            inv[gi, 8 + j] = 1.0 / (hi - lo)
    c["c_invedge"] = inv.reshape(1, 64)
    q = np.arange(128)
    c["c_blk"] = (q[:, None] // 8 == q[None, :] // 8).astype(np.float32)
    c["c_ltri"] = ((q[:, None] // 8 == q[None, :] // 8) & (q[:, None] % 8 < q[None, :] % 8)).astype(np.float32)
    c["c_io16"] = np.tile(np.arange(128, dtype=np.float32)[None, :], (128, NE))
    tok = (np.arange(NT, dtype=np.float32)[None, :] * 128 + np.arange(128, dtype=np.float32)[:, None])
    c["c_tokf"] = np.ascontiguousarray(np.repeat(tok[:, :, None], NE, axis=2)).reshape(128, NT * NE)
    _CONST.update(c)
    return _CONST


_NC = {}


def _get_nc():
    if "nc" not in _NC:
        _NC["nc"] = Builder().build()
    return _NC["nc"]


def kernel(x, norm1_g, w_in, w_fourier, w_pool, pool_scale, w_out, norm2_g, w_router, w_gate, w_up, w_down,
           final_g):
    f = lambda a: np.ascontiguousarray(np.asarray(a, dtype=np.float32))
    shared = {
        "norm1_g": f(norm1_g), "w_in": f(w_in), "w_fourier": f(w_fourier), "w_pool": f(w_pool),
        "pool_scale": f(pool_scale), "w_out": f(w_out), "norm2_g": f(norm2_g), "w_router": f(w_router),
        "w_gate": f(w_gate), "w_up": f(w_up), "w_down": f(w_down), "final_g": f(final_g).reshape(1, D),
    }
    shared.update(_constants())
    x = f(x)
    nc = _get_nc()
    in_maps = []
    for c in range(NCORES):
        m = dict(shared)
        m["x"] = x[c]
        in_maps.append(m)
    res = run_bass_kernel_spmd(nc, in_maps, core_ids=list(range(NCORES)))
    return np.stack([res.results[c]["out"] for c in range(NCORES)], axis=0).astype(np.float32)
```
